# Optimizing a Trainium2 kernel written in Bass

```python
import jax, jax.numpy as jnp
from jax import lax
import numpy as np


D_MODEL = 1024
BATCH = 8
SEQ = 2048
DEPTH = 2

N_ATTN_HEADS = 8
ATTN_HEAD_DIM = 64
ATTN_WIDTH = N_ATTN_HEADS * ATTN_HEAD_DIM
MOBA_BLOCK = 256
MOBA_TOPK = 3
Q_CHUNK = 16
CONV_WIDTH = 512
CONV_K = 3
N_BRANCH = 2
SPLIT_SIZES = (ATTN_WIDTH, ATTN_WIDTH, ATTN_WIDTH, CONV_WIDTH, CONV_WIDTH, CONV_WIDTH, D_MODEL, D_MODEL)
IN_COLS = sum(SPLIT_SIZES)
SPLIT_POINTS = tuple(int(s) for s in np.cumsum(SPLIT_SIZES)[:-1])
PEER_HEADS = 8
PEER_NKEYS = 128
PEER_EXPERTS = PEER_NKEYS * PEER_NKEYS
PEER_DKEY = 256
PEER_HALF = PEER_DKEY // 2
PEER_TOPK = 16
PEER_TOK_CHUNK = 128

NORM_EPS = 1e-6
MASK_VALUE = -1e30

kernel_name = "hybrid_moba_shortconv_peer_adaln"


def rmsnorm(x, g):
    xf = x.astype(jnp.float32)
    y = xf * lax.rsqrt(jnp.mean(xf * xf, axis=-1, keepdims=True) + NORM_EPS)
    return (y * g.astype(jnp.float32)).astype(x.dtype)


def alibi_slopes(n):
    return jnp.asarray(np.array([2.0 ** (-8.0 * (h + 1) / n) for h in range(n)], dtype=np.float32))


def moba_attention(q, k, v, slopes):
    B, S, H, hd = q.shape
    nb = -(-S // MOBA_BLOCK)
    pad = nb * MOBA_BLOCK - S
    ksel = min(MOBA_TOPK, nb - 1)
    scale = hd ** -0.5
    q = q.transpose(0, 2, 1, 3)
    k = jnp.pad(k.transpose(0, 2, 1, 3), ((0, 0), (0, 0), (0, pad), (0, 0)))
    v = jnp.pad(v.transpose(0, 2, 1, 3), ((0, 0), (0, 0), (0, pad), (0, 0)))
    kb = k.reshape(B, H, nb, MOBA_BLOCK, hd)
    vb = v.reshape(B, H, nb, MOBA_BLOCK, hd)
    qblk = jnp.arange(S, dtype=jnp.int32) // MOBA_BLOCK
    nc = S // Q_CHUNK

    def chunks(a):
        return jnp.moveaxis(a.reshape(B, H, nc, Q_CHUNK, *a.shape[3:]), 2, 0)

    xs = {"q": chunks(q), "start": jnp.arange(nc, dtype=jnp.int32) * Q_CHUNK}
    if ksel > 0:
        kmean = jnp.mean(kb.astype(jnp.float32), axis=3)
        gate = jnp.einsum("bhtd,bhnd->bhtn", q.astype(jnp.float32), kmean)
        past = jnp.arange(nb, dtype=jnp.int32)[None, :] < qblk[:, None]
        gate = jnp.where(past, gate, MASK_VALUE)
        _, idx = lax.top_k(gate, ksel)
        valid = idx < qblk[:, None]
        xs["idx"] = chunks(idx)
        xs["valid"] = chunks(valid)
    bi = jnp.arange(B)[:, None, None, None]
    hi = jnp.arange(H)[None, :, None, None]
    offs = jnp.arange(MOBA_BLOCK, dtype=jnp.int32)

    def body(xc):
        qc = xc["q"]
        start = xc["start"]
        pos_q = start + jnp.arange(Q_CHUNK, dtype=jnp.int32)
        own = start // MOBA_BLOCK
        k_own = lax.dynamic_index_in_dim(kb, own, axis=2, keepdims=False)
        v_own = lax.dynamic_index_in_dim(vb, own, axis=2, keepdims=False)
        d_own = pos_q[:, None] - (own * MOBA_BLOCK + offs)[None, :]
        s_own = (jnp.einsum("bhtd,bhsd->bhts", qc, k_own).astype(jnp.float32) * scale
                 - slopes[:, None, None] * d_own.astype(jnp.float32))
        s_own = jnp.where(d_own >= 0, s_own, MASK_VALUE)
        if ksel == 0:
            p = jax.nn.softmax(s_own, axis=-1).astype(v.dtype)
            return jnp.einsum("bhts,bhsd->bhtd", p, v_own)
        idxc = xc["idx"]
        validc = xc["valid"]
        k_g = kb[bi, hi, idxc]
        v_g = vb[bi, hi, idxc]
        pos_sel = idxc[..., None] * MOBA_BLOCK + offs
        d_sel = pos_q[None, None, :, None, None] - pos_sel
        s_sel = (jnp.einsum("bhtd,bhtjsd->bhtjs", qc, k_g).astype(jnp.float32) * scale
                 - slopes[None, :, None, None, None] * d_sel.astype(jnp.float32))
        s_sel = jnp.where(validc[..., None], s_sel, MASK_VALUE)
        s_all = jnp.concatenate([s_sel.reshape(B, H, Q_CHUNK, ksel * MOBA_BLOCK), s_own], axis=-1)
        p = jax.nn.softmax(s_all, axis=-1).astype(v.dtype)
        p_sel = p[..., : ksel * MOBA_BLOCK].reshape(B, H, Q_CHUNK, ksel, MOBA_BLOCK)
        p_own = p[..., ksel * MOBA_BLOCK:]
        return (jnp.einsum("bhts,bhsd->bhtd", p_own, v_own)
                + jnp.einsum("bhtjs,bhtjsd->bhtd", p_sel, v_g))

    out = lax.map(body, xs)
    return out.transpose(1, 0, 3, 2, 4).reshape(B, S, H * hd)


def gated_short_conv(b_gate, c_gate, h, w_conv):
    z = c_gate * h
    y = lax.conv_general_dilated(z, w_conv[:, None, :], window_strides=(1,),
                                 padding=[(CONV_K - 1, 0)],
                                 dimension_numbers=("NWC", "WIO", "NWC"),
                                 feature_group_count=CONV_WIDTH)
    return b_gate * y


def peer(h, w_query, sub_keys, expert_u, expert_v):
    B, S, D = h.shape
    T = B * S
    hf = h.reshape(T, D)
    q = (hf @ w_query).reshape(T, PEER_HEADS, 2, PEER_HALF)
    scores = jnp.einsum("thpd,hpnd->thpn", q, sub_keys).astype(jnp.float32)
    top_v, top_i = lax.top_k(scores, PEER_TOPK)
    cand_v = top_v[:, :, 0, :, None] + top_v[:, :, 1, None, :]
    cand_i = top_i[:, :, 0, :, None] * PEER_NKEYS + top_i[:, :, 1, None, :]
    cand_v = cand_v.reshape(T, PEER_HEADS, PEER_TOPK * PEER_TOPK)
    cand_i = cand_i.reshape(T, PEER_HEADS, PEER_TOPK * PEER_TOPK)
    best_v, best_pos = lax.top_k(cand_v, PEER_TOPK)
    expert_idx = jnp.take_along_axis(cand_i, best_pos, axis=-1)
    gates = jax.nn.softmax(best_v, axis=-1).astype(h.dtype)
    nc = T // PEER_TOK_CHUNK
    xs = {"h": hf.reshape(nc, PEER_TOK_CHUNK, D),
          "idx": expert_idx.reshape(nc, PEER_TOK_CHUNK, PEER_HEADS, PEER_TOPK),
          "g": gates.reshape(nc, PEER_TOK_CHUNK, PEER_HEADS, PEER_TOPK)}

    def body(xc):
        u = jnp.take(expert_u, xc["idx"], axis=0)
        vv = jnp.take(expert_v, xc["idx"], axis=0)
        a = jax.nn.gelu(jnp.einsum("td,thkd->thk", xc["h"], u))
        return jnp.einsum("thk,thkd->td", xc["g"] * a, vv)

    return lax.map(body, xs).reshape(B, S, D)


def setup_inputs(seed: int = 0) -> dict:
    key = jax.random.key(seed)
    ks = jax.random.split(key, 16)
    L, D = DEPTH, D_MODEL

    def nrm(k, shape, s):
        return jax.random.normal(k, shape, jnp.float32) * s

    return {
        "x": nrm(ks[0], (BATCH, SEQ, D), 1.0),
        "c": nrm(ks[1], (BATCH, D), 1.0),
        "norm1_g": 1.0 + nrm(ks[2], (L, D), 0.02),
        "norm2_g": 1.0 + nrm(ks[3], (L, D), 0.02),
        "w_ada": nrm(ks[4], (L, D, 6 * D), 0.5 * D ** -0.5),
        "b_ada": nrm(ks[5], (L, 6 * D), 0.02),
        "w_in": nrm(ks[6], (L, D, IN_COLS), D ** -0.5),
        "conv_w": nrm(ks[7], (L, CONV_K, CONV_WIDTH), CONV_K ** -0.5),
        "w_attn_proj": nrm(ks[8], (L, ATTN_WIDTH, D), ATTN_WIDTH ** -0.5),
        "w_conv_proj": nrm(ks[9], (L, CONV_WIDTH, D), CONV_WIDTH ** -0.5),
        "w_out": nrm(ks[10], (L, D, D), D ** -0.5),
        "w_query": nrm(ks[11], (L, D, PEER_HEADS * PEER_DKEY), D ** -0.5),
        "sub_keys": nrm(ks[12], (L, PEER_HEADS, 2, PEER_NKEYS, PEER_HALF), PEER_HALF ** -0.5),
        "expert_u": nrm(ks[13], (L, PEER_EXPERTS, D), D ** -0.5),
        "expert_v": nrm(ks[14], (L, PEER_EXPERTS, D), PEER_HEADS ** -0.5),
        "final_g": 1.0 + nrm(ks[15], (D,), 0.02),
    }


def reference(x, c, norm1_g, norm2_g, w_ada, b_ada, w_in, conv_w, w_attn_proj, w_conv_proj,
              w_out, w_query, sub_keys, expert_u, expert_v, final_g):
    B, S, D = x.shape
    slopes = alibi_slopes(N_ATTN_HEADS)
    for l in range(DEPTH):
        ada = (c @ w_ada[l] + b_ada[l])[:, None, :]
        sh1, sc1, g1, sh2, sc2, g2 = jnp.split(ada, 6, axis=-1)
        h = rmsnorm(x, norm1_g[l]) * (1.0 + sc1) + sh1
        proj = h @ w_in[l]
        q, k, v, b_gate, c_gate, hc, gate_a, gate_c = jnp.split(proj, SPLIT_POINTS, axis=-1)
        ya = moba_attention(q.reshape(B, S, N_ATTN_HEADS, ATTN_HEAD_DIM),
                            k.reshape(B, S, N_ATTN_HEADS, ATTN_HEAD_DIM),
                            v.reshape(B, S, N_ATTN_HEADS, ATTN_HEAD_DIM), slopes)
        yc = gated_short_conv(b_gate, c_gate, hc, conv_w[l])
        merged = (jax.nn.sigmoid(gate_a) * (ya @ w_attn_proj[l])
                  + jax.nn.sigmoid(gate_c) * (yc @ w_conv_proj[l]))
        x = x + g1 * (merged @ w_out[l])
        h = rmsnorm(x, norm2_g[l]) * (1.0 + sc2) + sh2
        x = x + g2 * peer(h, w_query[l], sub_keys[l], expert_u[l], expert_v[l])
    return rmsnorm(x, final_g)
```

```python
import numpy as np
from contextlib import ExitStack
import concourse.bass as bass
import concourse.mybir as mybir
from concourse.bass_utils import run_bass_kernel_spmd

F32 = mybir.dt.float32
BF16 = mybir.dt.bfloat16
I32 = mybir.dt.int32
U32 = mybir.dt.uint32
AF = mybir.ActivationFunctionType
ALU = mybir.AluOpType
AX = mybir.AxisListType

L = 2
T = 2048
D = 1024
NT = 16
NEG = -30000.0


class Prog:
    ENGS = ["pe", "act", "dve", "pool", "sp"]
    EPOCH = 30000

    def __init__(self, nc, n_dma_sems=32):
        self.nc = nc
        self.streams = {e: [] for e in self.ENGS}
        self.tick = {e: 0 for e in self.ENGS}
        self.ticksems = {e: [] for e in self.ENGS}
        self.dsems = [nc.alloc_semaphore(name=f"dq{i}") for i in range(n_dma_sems)]
        self.dtarget = [0] * n_dma_sems
        self.dnext = 0
        self.hsems = [nc.alloc_semaphore(name=f"hq{i}") for i in range(16)]
        self.htarget = [0] * 16
        self.hnext = 0
        self.state = {}
        self.seen = {e: {} for e in self.ENGS}
        self.semname = {}
        self.bsems = [nc.alloc_semaphore(name=f"bg{i}") for i in range(16)]
        self.btarget = [0] * 16
        self.bnext = 0
        self.bg_keys = set()

    def _ticket(self, eng):
        self.tick[eng] += 1
        ep = (self.tick[eng] - 1) // self.EPOCH
        while len(self.ticksems[eng]) <= ep:
            self.ticksems[eng].append(self.nc.alloc_semaphore(name=f"tk_{eng}_{len(self.ticksems[eng])}"))
        return (("t", eng, ep), self.tick[eng] - ep * self.EPOCH)

    def _sem(self, key):
        if key[0] == "t":
            return self.ticksems[key[1]][key[2]]
        if key[0] == "b":
            return self.bsems[key[1]]
        if key[0] == "h":
            return self.hsems[key[1]]
        return self.dsems[key[1]]

    def op(self, eng, fn, reads=(), writes=(), dma=False, bg=False):
        deps = {}

        def add(tk, own_ok):
            if tk is None:
                return
            key, val = tk
            if key[0] == "t" and key[1] == eng:
                if eng == "pe" or not own_ok:
                    return
            if deps.get(key, 0) < val:
                deps[key] = val

        for k in reads:
            st = self.state.get(k)
            if st:
                add(st["w"], True)
        for k in writes:
            st = self.state.get(k)
            if st:
                add(st["w"], True)
                for r in st["r"]:
                    add(r, True)
        if dma and bg:
            si = self.bnext
            self.bnext = (self.bnext + 1) % len(self.bsems)
            if self.btarget[si] > 0:
                add((("b", si), self.btarget[si]), True)
            self.bg_keys.update(writes)
        elif dma and eng == "sp":
            si = self.hnext
            self.hnext = (self.hnext + 1) % len(self.hsems)
            if self.htarget[si] > 0:
                add((("h", si), self.htarget[si]), True)
        elif dma:
            si = self.dnext
            self.dnext = (self.dnext + 1) % len(self.dsems)
            if self.dtarget[si] > 0:
                add((("d", si), self.dtarget[si]), True)
        waits = []
        for key, val in deps.items():
            if self.seen[eng].get(key, 0) >= val:
                continue
            self.seen[eng][key] = val
            waits.append((key, val))
        if dma and bg:
            self.btarget[si] += 16
            tk = (("b", si), self.btarget[si])
            inc = (("b", si), 16)
        elif dma and eng == "sp":
            self.htarget[si] += 16
            tk = (("h", si), self.htarget[si])
            inc = (("h", si), 16)
        elif dma:
            self.dtarget[si] += 16
            tk = (("d", si), self.dtarget[si])
            inc = (("d", si), 16)
        else:
            tk = self._ticket(eng)
            inc = (tk[0], 1)
        for k in reads:
            self.state.setdefault(k, {"w": None, "r": []})["r"].append(tk)
        for k in writes:
            st = self.state.setdefault(k, {"w": None, "r": []})
            st["w"] = tk
            st["r"] = []
        self.streams[eng].append((waits, fn, inc))
        return tk

    def fence(self):
        tks = []
        for e in self.ENGS:
            if self.tick[e] > 0:
                ep = (self.tick[e] - 1) // self.EPOCH
                tks.append((("t", e, ep), self.tick[e] - ep * self.EPOCH))
        for si, tv in enumerate(self.dtarget):
            if tv > 0:
                tks.append((("d", si), tv))
        for si, tv in enumerate(self.htarget):
            if tv > 0:
                tks.append((("h", si), tv))
        for e in self.ENGS:
            waits = []
            for key, val in tks:
                if key[0] == "t" and key[1] == e:
                    continue
                if self.seen[e].get(key, 0) >= val:
                    continue
                self.seen[e][key] = val
                waits.append((key, val))
            if waits:
                self.streams[e].append((waits, None, None))
        self.state = {k: v for k, v in self.state.items() if k in self.bg_keys}

    def emit(self):
        nc = self.nc
        with nc.Block() as block:
            def body(ename):
                def run(eng):
                    for waits, fn, inc in self.streams[ename]:
                        for key, val in waits:
                            eng.wait_ge(self._sem(key), val)
                        if fn is None:
                            continue
                        ins = fn(eng)
                        ins.then_inc(self._sem(inc[0]), inc[1])
                return run
            block.tensor(body("pe"))
            block.scalar(body("act"))
            block.vector(body("dve"))
            block.gpsimd(body("pool"))
            block.sync(body("sp"))


def mmgroup(items):
    def fn(e):
        ins = None
        for (o, l, r, s, t) in items:
            ins = e.matmul(o, l, r, start=s, stop=t)
        return ins
    return fn


def trgroup(items):
    def fn(e):
        ins = None
        for (o, i, idn) in items:
            ins = e.transpose(o, i, idn)
        return ins
    return fn


def dma(out, in_):
    return lambda e: e.dma_start(out=out, in_=in_)


def build(nlayers=L, dbg=False, dbg_layer=0, late=False):
    nc = bass.Bass("TRN2", target_bir_lowering=False)
    es = ExitStack()

    def din(name, shape, dt=F32):
        return nc.dram_tensor(name, list(shape), dt, kind="ExternalInput").ap()

    x_d = din("x", [T, D])
    crep_d = din("crep", [D, 128])
    w_ada_d = din("w_ada", [L, D, 6 * D])
    b_ada_d = din("b_ada_rep", [L, 128, 6 * D])
    n1g_d = din("n1g_rep", [L, 128, D])
    n2g_d = din("n2g_rep", [L, 128, D])
    fg_d = din("fg_rep", [128, D])
    w_in_d = din("w_in", [L, D, 5120])
    w_in_t_d = din("w_in_t", [L, 40, 128, 1024])
    wq_t_d = din("w_query_t", [L, 16, 128, 1024])
    convw_d = din("conv_wT", [L, 128, 12])
    wap_d = din("w_attn_proj", [L, 512, D])
    wcp_d = din("w_conv_proj", [L, 512, D])
    wout_d = din("w_out", [L, D, D])
    wq_d = din("w_query", [L, D, 2048])
    keysT_d = din("keysT", [L, 128, 2048])
    eall_d = din("experts_all", [L * 16384, 2 * D])
    ebf_d = nc.dram_tensor("ebf", [L * 16384, 2 * D], BF16, kind="Internal").ap()
    ident_d = din("c_ident", [128, 128])
    selB_d = din("c_selB", [128, 2048])
    cm_d = din("c_causal", [128, 2048])
    ar_d = din("c_alibi_rows", [4, 4, T])
    kb_d = din("c_kb", [128, 128])
    iota_d = din("c_iota16", [128, 16])
    out_d = nc.dram_tensor("out", [T, D], F32, kind="ExternalOutput").ap()
    dbg_d = {}
    if late:
        dbg_d["d_eidx"] = nc.dram_tensor("d_eidx", [128, 2048], I32, kind="ExternalOutput").ap()
        dbg_d["d_gates"] = nc.dram_tensor("d_gates", [128, 2048], F32, kind="ExternalOutput").ap()
    if dbg:
        for nm, shp, dt in [("d_hT", [128, 16384], BF16), ("d_yaT", [128, 8192], BF16), ("d_ycT", [128, 8192], BF16),
                            ("d_x1", [128, 16384], F32), ("d_eidx", [128, 2048], I32), ("d_gates", [128, 2048], F32),
                            ("d_x2", [128, 16384], F32), ("d_x0", [128, 16384], F32), ("d_ada", [128, 6144], F32), ("d_v", [128, 8192], BF16)]:
            dbg_d[nm] = nc.dram_tensor(nm, shp, dt, kind="ExternalOutput").ap()

    def sb(name, shape, dt):
        return es.enter_context(nc.sbuf_tensor(name, list(shape), dt))

    X = sb("X", [128, NT, D], F32)
    ADA = sb("ADA", [128, 6 * D], F32)
    HT = sb("HT", [128, 16384], BF16)
    R1 = sb("R1", [128, 16384], BF16)
    YA = sb("YA", [128, 8192], BF16)
    YC = sb("YC", [128, 8192], BF16)
    S = sb("S", [128, 4096], BF16)
    W = sb("W", [128, 4096], BF16)
    ident = sb("ident", [128, 128], BF16)
    onesb = sb("onesb", [128, 128], BF16)
    kb = sb("kb", [128, 128], F32)
    iota16 = sb("iota16", [128, 16], F32)
    small = sb("small", [128, 256], F32)
    cw = sb("cw", [128, 12], F32)
    smallR = sb("smallR", [128, 2 * 3 * 96], F32)
    tails = sb("tails", [128, 512], F32)
    iotx = sb("iotx", [128, 16], F32)
    PS = es.enter_context(nc.psum_tensor("PS", [128, 8, 512], F32))

    P = Prog(nc)
    Xv = X[:]
    ADAv = ADA[:]
    hT = HT[:].rearrange("p (c t) -> p c t", c=8)
    PSv = PS[:]

    def psbf(bank):
        return PSv[:, bank, :].bitcast(BF16)

    ss = small[:, 0:16]
    srt = small[:, 16:32]
    rstd = small[:, 32:48]

    P.op("pool", dma(ident[:], ident_d), writes=["ident"], dma=True)
    P.op("sp", dma(kb[:], kb_d), writes=["kb"], dma=True)
    P.op("sp", dma(iota16[:], iota_d), writes=["iota16"], dma=True)
    P.op("pool", lambda e: e.memset(onesb[:], 1.0), writes=["onesb"])
    P.op("dve", lambda e: e.tensor_scalar(out=iotx[:], in0=iota16[:], scalar1=16.0, scalar2=None, op0=ALU.mult),
         reads=["iota16"], writes=["iotx"])
    for tt in range(NT):
        P.op("sp", dma(Xv[:, tt, :], x_d[tt * 128:(tt + 1) * 128, :]), writes=[("x", tt)], dma=True)

    CR = 512
    ncast = (nlayers * 16384) // CR
    cast_i = [0]

    def bg_pump(n=1):
        for _ in range(n):
            ck = cast_i[0]
            if ck >= ncast:
                return
            cast_i[0] += 1
            P.op("pool", dma(ebf_d[ck * CR:(ck + 1) * CR, :], eall_d[ck * CR:(ck + 1) * CR, :]),
                 writes=[("ebf", ck)], dma=True, bg=True)

    pump_n = [0]

    Wt = [W[:, j * 1024:(j + 1) * 1024].rearrange("p (k n) -> p k n", k=8) for j in range(4)]
    Wwhole = W[:].rearrange("p (k n) -> p k n", k=8)
    wkeys_all = [("W4", j) for j in range(4)]
    wrot = [0]

    def next_w():
        j = wrot[0] % 4
        wrot[0] += 1
        return j

    def load_wtile(j, src):
        P.op("pool", dma(W[:, j * 1024:(j + 1) * 1024], src), writes=[("W4", j)], dma=True)
        bg_pump(pump_n[0])

    def norm_phase(A_ap, B_ap, hrow_of, keep_rows):
        ycf = YC[:].bitcast(F32)
        tmpn = [ycf[:, 0:1024], ycf[:, 1024:2048]]
        junk = YC[:, 4096:5120]
        for tt in range(NT):
            P.op("act", lambda e, tt=tt: e.activation(out=junk, in_=Xv[:, tt, :], func=AF.Square,
                                                     accum_out=ss[:, tt:tt + 1]),
                 reads=[("x", tt)], writes=["junk", ("ss", tt)])
        P.op("act", lambda e: e.activation(out=srt, in_=ss, func=AF.Sqrt, scale=1.0 / D, bias=1e-6),
             reads=[("ss", tt) for tt in range(NT)], writes=["srt"])
        P.op("dve", lambda e: e.reciprocal(out=rstd, in_=srt), reads=["srt"], writes=["rstd"])
        for tt in range(NT):
            tb = tt % 2
            hr = hrow_of(tt)
            P.op("dve", lambda e, tt=tt, tb=tb: e.scalar_tensor_tensor(
                out=tmpn[tb], in0=Xv[:, tt, :], scalar=rstd[:, tt:tt + 1], in1=A_ap, op0=ALU.mult, op1=ALU.mult),
                reads=[("x", tt), "rstd", "adaA"], writes=[("tmpn", tb)])
            P.op("pool", lambda e, tb=tb, hr=hr: e.tensor_tensor(out=hr[0], in0=tmpn[tb], in1=B_ap, op=ALU.add),
                 reads=[("tmpn", tb), "adaB"], writes=[hr[1]])
            bank = tt % 2
            pb = psbf(bank)
            P.op("pe", trgroup([(pb[:, c * 128:(c + 1) * 128], hr[0][:, c * 128:(c + 1) * 128], ident[:])
                                for c in range(8)]),
                 reads=[hr[1], "ident"], writes=[("ps", bank)])
            P.op("act", lambda e, tt=tt, pb=pb: e.copy(out=hT[:, :, tt * 128:(tt + 1) * 128],
                                                      in_=pb.rearrange("p (c t) -> p c t", c=8)),
                 reads=[("ps", bank)], writes=[("hT", tt)])

    for l in range(nlayers):
        P.fence()
        if dbg and l == dbg_layer:
            P.op("sp", dma(dbg_d["d_x0"], X[:].rearrange("p t d -> p (t d)")), reads=[], writes=["dbg_x0"], dma=True)
            P.fence()
        wst = HT[:].bitcast(F32).rearrange("p (b k n) -> p b k n", b=2, k=8)
        crep = R1[:, 0:2048].bitcast(F32).rearrange("p (k m) -> p k m", k=8)
        brep = R1[:, 2048:4096].bitcast(F32).rearrange("p (b n) -> p b n", b=2)
        grep = R1[:, 4096:8192].bitcast(F32).rearrange("p (b n) -> p b n", b=2)
        P.op("sp", dma(crep, crep_d.rearrange("(k p) m -> p k m", p=128)), writes=["crep"], dma=True)
        P.op("sp", dma(grep[:, 0, :], n1g_d[l]), writes=[("grep", 0)], dma=True)
        P.op("sp", dma(grep[:, 1, :], n2g_d[l]), writes=[("grep", 1)], dma=True)
        P.op("sp", dma(cw[:], convw_d[l]), writes=["cw"], dma=True)
        wada_v = w_ada_d[l].rearrange("(k p) n -> p k n", p=128)
        for nt in range(12):
            b = nt % 2
            P.op("sp", dma(wst[:, b], wada_v[:, :, nt * 512:(nt + 1) * 512]), writes=[("wst", b)], dma=True)
            P.op("sp", dma(brep[:, b, :], b_ada_d[l][:, nt * 512:(nt + 1) * 512]), writes=[("brep", b)], dma=True)
            P.op("pe", mmgroup([(PSv[:, b, :], crep[:, k, :], wst[:, b, k, :], k == 0, k == 7) for k in range(8)]),
                 reads=[("wst", b), "crep"], writes=[("ps", b)])
            P.op("dve", lambda e, b=b, nt=nt: e.tensor_tensor(out=ADAv[:, nt * 512:(nt + 1) * 512], in0=PSv[:, b, :],
                                                              in1=brep[:, b, :], op=ALU.add),
                 reads=[("ps", b), ("brep", b)], writes=[("ada", nt)])
        P.op("dve", lambda e: e.scalar_tensor_tensor(out=ADAv[:, 1024:2048], in0=ADAv[:, 1024:2048], scalar=1.0,
                                                     in1=grep[:, 0, :], op0=ALU.add, op1=ALU.mult),
             reads=[("ada", 2), ("ada", 3), ("grep", 0)], writes=[("ada", 2), ("ada", 3)])
        P.op("dve", lambda e: e.scalar_tensor_tensor(out=ADAv[:, 4096:5120], in0=ADAv[:, 4096:5120], scalar=1.0,
                                                     in1=grep[:, 1, :], op0=ALU.add, op1=ALU.mult),
             reads=[("ada", 8), ("ada", 9), ("grep", 1)], writes=[("ada", 8), ("ada", 9)])
        if dbg and l == dbg_layer:
            P.op("sp", dma(dbg_d["d_ada"], ADAv), reads=[("ada", i) for i in range(12)], writes=["dbg_ada"], dma=True)
        P.fence()
        SH1, A1, G1 = ADAv[:, 0:1024], ADAv[:, 1024:2048], ADAv[:, 2048:3072]
        SH2, A2, G2 = ADAv[:, 3072:4096], ADAv[:, 4096:5120], ADAv[:, 5120:6144]

        hrows = [YC[:, 5120:6144], YC[:, 6144:7168]]
        norm_phase(A1, SH1, lambda tt: (hrows[tt % 2], ("hrow", tt % 2)), False)
        P.fence()
        if dbg and l == dbg_layer:
            P.op("sp", dma(dbg_d["d_hT"], HT[:]), reads=[("hT", tt) for tt in range(NT)], writes=["dbg_hT"], dma=True)
        win = w_in_d[l]
        v_sb = R1[:, 0:8192].rearrange("p (t n) -> p t n", t=16)
        qslots = R1[:, 8192:16384].rearrange("p (s t) -> p s t", s=4)
        qz = [qslots[:, 1, :], qslots[:, 2, :]]
        P.op("pool", lambda e: e.memset(qz[0][64:128, :], 0.0), writes=[("qzero", 0)])
        P.op("pool", lambda e: e.memset(qz[1][0:64, :], 0.0), writes=[("qzero", 1)])
        P.op("pool", dma(Wwhole, win[:, 1024:1536].rearrange("(k p) n -> p k n", p=128)), writes=wkeys_all, dma=True)
        for tt in range(NT):
            bank = tt % 2
            P.op("pe", mmgroup([(PSv[:, bank, :], hT[:, k, tt * 128:(tt + 1) * 128], Wwhole[:, k, :], k == 0, k == 7)
                                for k in range(8)]),
                 reads=[("hT", tt)] + wkeys_all, writes=[("ps", bank)])
            P.op("act", lambda e, tt=tt, bank=bank: e.copy(out=v_sb[:, tt, :], in_=PSv[:, bank, :]),
                 reads=[("ps", bank)], writes=[("v", tt)])
        if dbg and l == dbg_layer:
            P.op("sp", dma(dbg_d["d_v"], R1[:, 0:8192]), reads=[("v", tt) for tt in range(NT)], writes=["dbg_v"], dma=True)
        Rc = [YC[:, 0:2048], YC[:, 2048:4096]]
        selB = YC[:, 4096:6144].rearrange("p (j m) -> p j m", j=16)
        CM = YC[:, 6144:8192].rearrange("p (j q) -> p j q", j=4)
        P.op("pool", dma(YC[:, 4096:6144], selB_d), writes=["selB"], dma=True)
        P.op("pool", dma(YC[:, 6144:8192], cm_d), writes=["CM"], dma=True)
        PT = [S[:, i * 512:(i + 1) * 512] for i in range(3)]
        rcp = S[:, 2048:3072].bitcast(F32)
        gsb = small[:, 64:80]
        m8 = small[:, 80:96]
        mb32 = small[:, 96:112]
        km32 = small[:, 112:120]
        mbb = S[:, 3072:3088]
        kmT = S[:, 3104:3112]
        yaT = YA[:].rearrange("p (c t) -> p c t", c=4)
        sbi = [0]
        pti = [0]
        pump_n[0] = 0
        for c in range(4):
            cb = c % 2
            kT = qslots[:, 0, :]
            jq = next_w()
            load_wtile(jq, w_in_t_d[l, c])
            jk = next_w()
            load_wtile(jk, w_in_t_d[l, 4 + c])
            if l == 0:
                bg_pump(8)
            P.op("pool", lambda e, cb=cb: e.memset(Rc[cb], 0.0),
                 writes=[("Rc", cb, tt) for tt in range(NT)] + [("RcA", cb)])
            P.op("pool", dma(Rc[cb][16:20, :], ar_d[c]), writes=[("RcA", cb)], dma=True)
            for Q in range(4):
                P.op("pe", mmgroup([(PSv[:, 6, :], Wt[jk][:, k, :], hT[:, k, Q * 512:(Q + 1) * 512], k == 0, k == 7)
                                    for k in range(8)]),
                     reads=[("W4", jk)] + [("hT", 4 * Q + i) for i in range(4)], writes=[("ps", 6)])
                P.op("act", lambda e, Q=Q, kT=kT: e.copy(out=kT[:, Q * 512:(Q + 1) * 512], in_=PSv[:, 6, :]),
                     reads=[("ps", 6)], writes=[("kT", 0, Q)])
                P.op("pe", mmgroup([(PSv[:, 7, :], Wt[jq][:, k, :], hT[:, k, Q * 512:(Q + 1) * 512], k == 0, k == 7)
                                    for k in range(8)]),
                     reads=[("W4", jq)] + [("hT", 4 * Q + i) for i in range(4)], writes=[("ps", 7)])
                for e8 in range(2):
                    P.op("act", lambda e, Q=Q, e8=e8: e.mul(out=qz[e8][64 * e8:64 * e8 + 64, Q * 512:(Q + 1) * 512],
                                                            in_=PSv[64 * e8:64 * e8 + 64, 7, :], mul=0.125),
                         reads=[("ps", 7)], writes=[("qT", 0, Q, e8)])
            P.op("dve", lambda e, kT=kT: e.tensor_reduce(out=km32, in_=kT.rearrange("p (n s) -> p n s", n=8),
                                                         axis=AX.X, op=ALU.add),
                 reads=[("kT", 0, Q) for Q in range(4)], writes=["km32"])
            P.op("dve", lambda e: e.tensor_scalar(out=kmT, in0=km32, scalar1=1.0 / 256, scalar2=None, op0=ALU.mult),
                 reads=["km32"], writes=["kmT"])
            for tt in range(8, NT):
                qb = tt // 2
                P.op("pe", mmgroup([(PSv[:, 5, e8 * 8:(e8 + 1) * 8],
                                     qz[e8][64 * e8:64 * e8 + 64, tt * 128:(tt + 1) * 128],
                                     kmT[64 * e8:64 * e8 + 64, :], True, True) for e8 in range(2)]),
                     reads=[("qT", 0, tt // 4, 0), ("qT", 0, tt // 4, 1), "kmT"], writes=[("ps", 5)])
                P.op("dve", lambda e: e.tensor_copy(out=gsb, in_=PSv[:, 5, 0:16]), reads=[("ps", 5)], writes=["gsb"])
                P.op("dve", lambda e, qb=qb: e.memset(gsb.rearrange("p (a n) -> p a n", a=2)[:, :, qb:8], -1e30),
                     reads=["gsb"], writes=["gsb"])
                for e8 in range(2):
                    P.op("dve", lambda e, e8=e8: e.max(out=m8[:, e8 * 8:(e8 + 1) * 8], in_=gsb[:, e8 * 8:(e8 + 1) * 8]),
                         reads=["gsb"], writes=[("m8", e8)])
                for e8 in range(2):
                    P.op("dve", lambda e, e8=e8: e.tensor_scalar(out=mb32[:, e8 * 8:(e8 + 1) * 8],
                                                                 in0=gsb[:, e8 * 8:(e8 + 1) * 8],
                                                                 scalar1=m8[:, e8 * 8 + 2:e8 * 8 + 3], scalar2=None,
                                                                 op0=ALU.is_ge),
                         reads=["gsb", ("m8", e8)], writes=[("mb32", e8)])
                P.op("dve", lambda e: e.tensor_scalar(out=mbb, in0=mb32, scalar1=-1.0, scalar2=-NEG, op0=ALU.add,
                                                      op1=ALU.mult),
                     reads=[("mb32", 0), ("mb32", 1)], writes=["mbb"])
                P.op("dve", lambda e, qb=qb: e.memset(mbb.rearrange("p (a n) -> p a n", a=2)[:, :, qb:8], 0.0),
                     reads=["mbb"], writes=["mbb"])
                pb5 = psbf(5)
                P.op("pe", trgroup([(pb5[0:16, 512:640], mbb, ident[:])]), reads=["mbb", "ident"], writes=[("ps", 5)])
                P.op("act", lambda e, tt=tt, cb=cb, pb5=pb5: e.copy(out=Rc[cb][0:16, tt * 128:(tt + 1) * 128],
                                                                   in_=pb5[0:16, 512:640]),
                     reads=[("ps", 5)], writes=[("Rc", cb, tt)])
            steps = [(e8, Q, kt) for Q in range(4) for e8 in range(2) for kt in range(4 * Q + 4)]
            pend = None

            def emit_pv(st):
                e8, Q, kt, ptb = st
                nkt = 4 * Q + 4
                lo, hi = 64 * e8, 64 * e8 + 64
                P.op("pe", mmgroup([(PSv[:, 3, :], v_sb[:, kt, c * 128:(c + 1) * 128], PT[ptb], kt == 0,
                                     kt == nkt - 1),
                                    (PSv[:, 4, :], onesb[:], PT[ptb], kt == 0, kt == nkt - 1)]),
                     reads=[("PT", ptb), ("v", kt), "onesb"], writes=[("ps", 3), ("ps", 4)])
                if kt == nkt - 1:
                    P.op("dve", lambda e, lo=lo, hi=hi: e.reciprocal(out=rcp[lo:hi, :], in_=PSv[lo:hi, 4, :]),
                         reads=[("ps", 4)], writes=["rcp"])
                    P.op("dve", lambda e, lo=lo, hi=hi, Q=Q, c=c: e.tensor_tensor(
                        out=yaT[lo:hi, c, Q * 512:(Q + 1) * 512], in0=PSv[lo:hi, 3, :], in1=rcp[lo:hi, :], op=ALU.mult),
                        reads=[("ps", 3), "rcp"], writes=[("yaT", c, Q, e8)])

            for (e8, Q, kt) in steps:
                h = 2 * c + e8
                lo, hi = 64 * e8, 64 * e8 + 64
                n = kt // 2
                sbk = sbi[0] % 3
                sbi[0] += 1
                ptb = pti[0] % 3
                pti[0] += 1
                items = [(PSv[:, sbk, :], kT[:, kt * 128:(kt + 1) * 128], qz[e8][:, Q * 512:(Q + 1) * 512],
                          True, False),
                         (PSv[:, sbk, :], selB[:, e8 * 8 + n, :], Rc[cb][:, Q * 512:(Q + 1) * 512], False,
                          kt < 4 * Q)]
                if kt >= 4 * Q:
                    items.append((PSv[:, sbk, :], ident[:], CM[:, kt - 4 * Q, :], False, True))
                P.op("pe", mmgroup(items),
                     reads=[("kT", 0, kt // 4), ("qT", 0, Q, e8), ("qzero", e8), "selB", "CM", "ident", ("RcA", cb)] +
                           [("Rc", cb, 4 * Q + i) for i in range(4)],
                     writes=[("ps", sbk)])
                P.op("act", lambda e, sbk=sbk, ptb=ptb, h=h, kt=kt: e.activation(
                    out=PT[ptb], in_=PSv[:, sbk, :], func=AF.Exp, bias=kb[:, h * 16 + kt:h * 16 + kt + 1],
                    scale=1.0),
                    reads=[("ps", sbk), "kb"], writes=[("PT", ptb)])
                if pend is not None:
                    emit_pv(pend)
                pend = (e8, Q, kt, ptb)
            emit_pv(pend)
        P.fence()
        if dbg and l == dbg_layer:
            P.op("sp", dma(dbg_d["d_yaT"], YA[:]), reads=[], writes=["dbg_ya"], dma=True)
            P.fence()

        pump_n[0] = 0
        r1f = R1[:].bitcast(F32)
        zf = r1f[:, 0:2050]
        yf = r1f[:, 2304:4352]
        hcs = [r1f[:, 4608:5120], r1f[:, 5120:5632]]
        ycT = YC[:].rearrange("p (c t) -> p c t", c=4)
        P.op("dve", lambda e: e.memset(zf[:, 0:2], 0.0), writes=["z0"])
        for cc in range(4):
            jc = next_w()
            load_wtile(jc, w_in_t_d[l, 16 + cc])
            jh = next_w()
            load_wtile(jh, w_in_t_d[l, 20 + cc])
            jb = next_w()
            load_wtile(jb, w_in_t_d[l, 12 + cc])
            for Q in range(4):
                hb = Q % 2
                bc_, bh_ = 4 * hb, 4 * hb + 1
                P.op("pe", mmgroup([(PSv[:, bc_, :], Wt[jc][:, k, :], hT[:, k, Q * 512:(Q + 1) * 512], k == 0, k == 7)
                                    for k in range(8)]), reads=[("W4", jc)], writes=[("ps", bc_)])
                P.op("pe", mmgroup([(PSv[:, bh_, :], Wt[jh][:, k, :], hT[:, k, Q * 512:(Q + 1) * 512], k == 0, k == 7)
                                    for k in range(8)]), reads=[("W4", jh)], writes=[("ps", bh_)])
                P.op("act", lambda e, hb=hb, bh_=bh_: e.copy(out=hcs[hb], in_=PSv[:, bh_, :]), reads=[("ps", bh_)],
                     writes=[("hcs", hb)])
                P.op("dve", lambda e, hb=hb, Q=Q, bc_=bc_: e.tensor_tensor(out=zf[:, 2 + Q * 512:2 + (Q + 1) * 512],
                                                                          in0=PSv[:, bc_, :], in1=hcs[hb], op=ALU.mult),
                     reads=[("ps", bc_), ("hcs", hb)], writes=[("z", Q)])
            zr = [("z", Q) for Q in range(4)] + ["z0"]
            P.op("dve", lambda e, cc=cc: e.tensor_scalar(out=yf, in0=zf[:, 2:2050], scalar1=cw[:, cc * 3 + 2:cc * 3 + 3],
                                                         scalar2=None, op0=ALU.mult), reads=zr + ["cw"], writes=["y"])
            P.op("dve", lambda e, cc=cc: e.scalar_tensor_tensor(out=yf, in0=zf[:, 1:2049],
                                                                scalar=cw[:, cc * 3 + 1:cc * 3 + 2], in1=yf,
                                                                op0=ALU.mult, op1=ALU.add), reads=zr + ["y"], writes=["y"])
            P.op("dve", lambda e, cc=cc: e.scalar_tensor_tensor(out=yf, in0=zf[:, 0:2048],
                                                                scalar=cw[:, cc * 3:cc * 3 + 1], in1=yf,
                                                                op0=ALU.mult, op1=ALU.add), reads=zr + ["y"], writes=["y"])
            for Q in range(4):
                bank = 2 + Q % 2
                P.op("pe", mmgroup([(PSv[:, bank, :], Wt[jb][:, k, :], hT[:, k, Q * 512:(Q + 1) * 512], k == 0, k == 7)
                                    for k in range(8)]), reads=[("W4", jb)], writes=[("ps", bank)])
                P.op("dve", lambda e, bank=bank, cc=cc, Q=Q: e.tensor_tensor(
                    out=ycT[:, cc, Q * 512:(Q + 1) * 512], in0=PSv[:, bank, :], in1=yf[:, Q * 512:(Q + 1) * 512],
                    op=ALU.mult), reads=[("ps", bank), "y"], writes=[("ycT", cc, Q)])
        P.fence()
        if dbg and l == dbg_layer:
            P.op("sp", dma(dbg_d["d_ycT"], YC[:]), reads=[], writes=["dbg_yc"], dma=True)
            P.fence()

        mT = R1[:].rearrange("p (c t) -> p c t", c=8)
        sf = S[:].bitcast(F32)
        sa = sf[:, 0:512]
        sc = sf[:, 512:1024]
        WAC = [S[:, 2048 + i * 512:2048 + (i + 1) * 512].rearrange("p (k n) -> p k n", k=4) for i in range(4)]
        wap_v = wap_d[l].rearrange("(k p) n -> p k n", p=128)
        wcp_v = wcp_d[l].rearrange("(k p) n -> p k n", p=128)
        it = 0
        for dc in range(8):
            jga = next_w()
            load_wtile(jga, w_in_t_d[l, 24 + dc])
            jgc = next_w()
            load_wtile(jgc, w_in_t_d[l, 32 + dc])
            wa = WAC[(dc % 2) * 2]
            wc = WAC[(dc % 2) * 2 + 1]
            P.op("pool", dma(wa, wap_v[:, :, dc * 128:(dc + 1) * 128]), writes=[("WAC", (dc % 2) * 2)], dma=True)
            P.op("pool", dma(wc, wcp_v[:, :, dc * 128:(dc + 1) * 128]), writes=[("WAC", (dc % 2) * 2 + 1)], dma=True)
            for Q in range(4):
                b0 = 4 * (it % 2)
                it += 1
                qs = slice(Q * 512, (Q + 1) * 512)
                P.op("pe", mmgroup([(PSv[:, b0, :], wa[:, k, :], yaT[:, k, qs], k == 0, k == 3) for k in range(4)]),
                     reads=[("WAC", (dc % 2) * 2)], writes=[("ps", b0)])
                P.op("pe", mmgroup([(PSv[:, b0 + 1, :], Wt[jga][:, k, :], hT[:, k, qs], k == 0, k == 7)
                                    for k in range(8)]), reads=[("W4", jga)], writes=[("ps", b0 + 1)])
                P.op("pe", mmgroup([(PSv[:, b0 + 2, :], wc[:, k, :], ycT[:, k, qs], k == 0, k == 3) for k in range(4)]),
                     reads=[("WAC", (dc % 2) * 2 + 1)], writes=[("ps", b0 + 2)])
                P.op("pe", mmgroup([(PSv[:, b0 + 3, :], Wt[jgc][:, k, :], hT[:, k, qs], k == 0, k == 7)
                                    for k in range(8)]), reads=[("W4", jgc)], writes=[("ps", b0 + 3)])
                P.op("act", lambda e, b0=b0: e.activation(out=sa, in_=PSv[:, b0 + 1, :], func=AF.Sigmoid),
                     reads=[("ps", b0 + 1)], writes=["sa"])
                P.op("act", lambda e, b0=b0: e.activation(out=sc, in_=PSv[:, b0 + 3, :], func=AF.Sigmoid),
                     reads=[("ps", b0 + 3)], writes=["sc"])
                P.op("dve", lambda e, b0=b0: e.tensor_tensor(out=sa, in0=sa, in1=PSv[:, b0, :], op=ALU.mult),
                     reads=["sa", ("ps", b0)], writes=["sa"])
                P.op("dve", lambda e, b0=b0: e.tensor_tensor(out=sc, in0=sc, in1=PSv[:, b0 + 2, :], op=ALU.mult),
                     reads=["sc", ("ps", b0 + 2)], writes=["sc"])
                P.op("pool", lambda e, dc=dc, qs=qs: e.tensor_tensor(out=mT[:, dc, qs], in0=sa, in1=sc, op=ALU.add),
                     reads=["sa", "sc"], writes=[("mT", dc, Q)])
        P.fence()
        otmp = [sf[:, 0:512], sf[:, 512:1024]]
        wout_v = wout_d[l].rearrange("(k p) n -> p k n", p=128)
        for half in range(2):
            hs = slice(half * 512, (half + 1) * 512)
            P.op("pool", dma(Wwhole, wout_v[:, :, hs]), writes=wkeys_all, dma=True)
            for tt in range(NT):
                bank = tt % 2
                P.op("pe", mmgroup([(PSv[:, bank, :], mT[:, k, tt * 128:(tt + 1) * 128], Wwhole[:, k, :], k == 0, k == 7)
                                    for k in range(8)]), reads=wkeys_all, writes=[("ps", bank)])
                P.op("dve", lambda e, bank=bank, hs=hs: e.tensor_tensor(out=otmp[bank], in0=PSv[:, bank, :],
                                                                        in1=G1[:, hs], op=ALU.mult),
                     reads=[("ps", bank)], writes=[("otmp", bank)])
                P.op("pool", lambda e, bank=bank, hs=hs, tt=tt: e.tensor_tensor(out=Xv[:, tt, hs], in0=Xv[:, tt, hs],
                                                                                in1=otmp[bank], op=ALU.add),
                     reads=[("otmp", bank), ("x", tt)], writes=[("x", tt)])
        P.fence()
        if dbg and l == dbg_layer:
            P.op("sp", dma(dbg_d["d_x1"], X[:].rearrange("p t d -> p (t d)")), reads=[], writes=["dbg_x1"], dma=True)
            P.fence()

        H2 = R1[:].rearrange("p (t d) -> p t d", t=16)
        norm_phase(A2, SH2, lambda tt: (H2[:, tt, :], ("h2", tt)), True)
        P.fence()
        keysT = S[:, 0:2048].rearrange("p (j n) -> p j n", j=16)
        P.op("pool", dma(S[:, 0:2048], keysT_d[l]), writes=["keysT"], dma=True)
        q2T = [YC[:, 0:4096].rearrange("p (w t) -> p w t", w=2), YC[:, 4096:8192].rearrange("p (w t) -> p w t", w=2)]
        eidx = YA[:, 0:4096].bitcast(I32).rearrange("p (t k) -> p t k", t=16)
        gates = YA[:, 4096:8192].bitcast(F32).rearrange("p (t k) -> p t k", t=16)
        wqv = wq_d[l]

        smR = smallR[:].rearrange("p (g c w) -> p g c w", g=2, c=3)

        def mk_scratch(cid, par):
            base = [S[:, 2048:4096].bitcast(F32), W[:, 2048:4096].bitcast(F32), ADAv[:, 3072:4096]][cid]
            sm = smR[:, par, cid, :]
            return dict(scs=base[:, 0:256], scw=base[:, 256:512], cand=base[:, 512:768], candw=base[:, 768:1024],
                        vv=sm[:, 0:32], ixu=sm[:, 32:64].bitcast(U32), bv=sm[:, 64:80],
                        posu=sm[:, 80:96].bitcast(U32), cid=cid, par=par)

        def chain(h, tt, sc, qb2):
            KS_ = lambda name, *a: (name, sc["cid"]) + tuple(a)
            K_ = lambda name, *a: (name, sc["cid"], sc["par"]) + tuple(a)
            scs, scw, cand, candw = sc["scs"], sc["scw"], sc["cand"], sc["candw"]
            vv, ixu, bv, posu = sc["vv"], sc["ixu"], sc["bv"], sc["posu"]
            bank = 2 + tt % 2
            ts_ = slice(tt * 128, (tt + 1) * 128)
            P.op("pe", mmgroup([(PSv[:, bank, p2 * 128:(p2 + 1) * 128], qb2[:, p2, ts_], keysT[:, 2 * h + p2, :],
                                 True, True) for p2 in range(2)]),
                 reads=[("q2T", h % 2, p2, tt // 4) for p2 in range(2)] + ["keysT"], writes=[("ps", bank)])
            P.op("act", lambda e: e.copy(out=scs, in_=PSv[:, bank, 0:256]), reads=[("ps", bank)], writes=[KS_("scs")])
            yield
            for p2 in range(2):
                sl = slice(p2 * 128, (p2 + 1) * 128)
                v0 = slice(p2 * 16, p2 * 16 + 8)
                v1 = slice(p2 * 16 + 8, p2 * 16 + 16)
                P.op("dve", lambda e, v0=v0, sl=sl: e.max(out=vv[:, v0], in_=scs[:, sl]), reads=[KS_("scs")], writes=[K_("vv", p2, 0)])
                yield
                P.op("dve", lambda e, v0=v0, sl=sl: e.max_index(out=ixu[:, v0], in_max=vv[:, v0], in_values=scs[:, sl]),
                     reads=[KS_("scs"), K_("vv", p2, 0)], writes=[K_("ix", p2, 0)])
                yield
                P.op("dve", lambda e, v0=v0, sl=sl: e.match_replace(out=scw[:, sl], in_to_replace=vv[:, v0], in_values=scs[:, sl],
                                                      imm_value=-1e30),
                     reads=[KS_("scs"), K_("vv", p2, 0)], writes=[KS_("scw", p2)])
                yield
                P.op("dve", lambda e, v1=v1, sl=sl: e.max(out=vv[:, v1], in_=scw[:, sl]), reads=[KS_("scw", p2)],
                     writes=[K_("vv", p2, 1)])
                yield
                P.op("dve", lambda e, v1=v1, sl=sl: e.max_index(out=ixu[:, v1], in_max=vv[:, v1], in_values=scw[:, sl]),
                     reads=[KS_("scw", p2), K_("vv", p2, 1)], writes=[K_("ix", p2, 1)])
                yield
            vkeys = [K_("vv", a, b) for a in range(2) for b in range(2)]
            ikeys = [K_("ix", a, b) for a in range(2) for b in range(2)]
            c3 = cand.rearrange("p (a b) -> p a b", a=16)
            P.op("dve", lambda e: e.tensor_tensor(out=c3, in0=vv[:, 0:16].unsqueeze(2).to_broadcast([128, 16, 16]),
                                                  in1=vv[:, 16:32].unsqueeze(1).to_broadcast([128, 16, 16]),
                                                  op=ALU.add), reads=vkeys, writes=[KS_("cand")])
            yield
            P.op("dve", lambda e: e.max(out=bv[:, 0:8], in_=cand), reads=[KS_("cand")], writes=[K_("bv", 0)])
            yield
            P.op("dve", lambda e: e.max_index(out=posu[:, 0:8], in_max=bv[:, 0:8], in_values=cand),
                 reads=[KS_("cand"), K_("bv", 0)], writes=[K_("pos", 0)])
            yield
            P.op("dve", lambda e: e.match_replace(out=candw, in_to_replace=bv[:, 0:8], in_values=cand,
                                                  imm_value=-1e30), reads=[KS_("cand"), K_("bv", 0)],
                 writes=[KS_("candw")])
            yield
            P.op("dve", lambda e: e.max(out=bv[:, 8:16], in_=candw), reads=[KS_("candw")], writes=[K_("bv", 1)])
            yield
            P.op("dve", lambda e: e.max_index(out=posu[:, 8:16], in_max=bv[:, 8:16], in_values=candw),
                 reads=[KS_("candw"), K_("bv", 1)], writes=[K_("pos", 1)])
            yield

        eqB = ADAv[:, 4096:4864]

        def tail(h, t0, n, par):
            KB = lambda name, *a: ("tl", name) + tuple(a)
            ck = lambda name, *a: [(name, c_, par) + tuple(a) for c_ in range(n)]
            sm = smR[:, par, 0:n, :]
            ixuB = sm[:, :, 32:64].bitcast(U32)
            bvB = sm[:, :, 64:80]
            posuB = sm[:, :, 80:96].bitcast(U32)
            m = n * 16
            posf = tails[:, 0:m]
            a16 = tails[:, 48:48 + m]
            bq = tails[:, 96:96 + m]
            i1 = tails[:, 144:144 + m]
            i2 = tails[:, 192:192 + m]
            ef = tails[:, 240:240 + m]
            ixf = tails[:, 288:288 + 2 * m].rearrange("p (c w) -> p c w", c=n)
            ex = tails[:, 384:384 + m].rearrange("p (c w) -> p c w", c=n)
            nm = tails[:, 432:432 + n]
            ssum = tails[:, 440:440 + n]
            rs = tails[:, 448:448 + n]
            eq2 = eqB[:, 0:m * 16].rearrange("p (k a) -> p k a", a=16)
            eq3 = eqB[:, 0:m * 16].rearrange("p (c k a) -> p c k a", c=n, a=16)
            v3 = lambda ap: ap.rearrange("p (c w) -> p c w", c=n)
            bc_k = lambda ap: ap.unsqueeze(2).to_broadcast([128, m, 16])
            bc_a = lambda ap: ap.unsqueeze(1).to_broadcast([128, m, 16])
            P.op("dve", lambda e: e.tensor_copy(out=v3(posf), in_=posuB), reads=ck("pos", 0) + ck("pos", 1),
                 writes=[KB("posf")])
            yield
            P.op("dve", lambda e: e.scalar_tensor_tensor(out=eq2, in0=bc_a(iota16[:]), scalar=16.0, in1=bc_k(posf),
                                                         op0=ALU.mult, op1=ALU.is_le),
                 reads=[KB("posf")], writes=[KB("eq")])
            yield
            P.op("dve", lambda e: e.tensor_reduce(out=a16, in_=eq2, axis=AX.X, op=ALU.add), reads=[KB("eq")],
                 writes=[KB("a16")])
            yield
            P.op("dve", lambda e: e.tensor_scalar(out=a16, in0=a16, scalar1=16.0, scalar2=-16.0, op0=ALU.mult,
                                                  op1=ALU.add), reads=[KB("a16")], writes=[KB("a16")])
            yield
            P.op("dve", lambda e: e.tensor_tensor(out=bq, in0=posf, in1=a16, op=ALU.subtract),
                 reads=[KB("posf"), KB("a16")], writes=[KB("bq")])
            yield
            P.op("dve", lambda e: e.tensor_copy(out=ixf, in_=ixuB),
                 reads=[k_ for a_ in range(2) for b_ in range(2) for k_ in ck("ix", a_, b_)], writes=[KB("ixf")])
            yield
            P.op("dve", lambda e, loff=float(l * 16384): e.tensor_scalar(
                out=ixf[:, :, 0:16], in0=ixf[:, :, 0:16], scalar1=128.0, scalar2=loff, op0=ALU.mult, op1=ALU.add),
                 reads=[KB("ixf")], writes=[KB("ixf")])
            yield
            for which in range(2):
                src = a16 if which == 0 else bq
                mul = 16.0 if which == 0 else 1.0
                dst = i1 if which == 0 else i2
                P.op("dve", lambda e, src=src, mul=mul: e.scalar_tensor_tensor(
                    out=eq2, in0=bc_a(iota16[:]), scalar=mul, in1=bc_k(src), op0=ALU.mult, op1=ALU.is_equal),
                    reads=[KB("a16"), KB("bq"), KB("eq")], writes=[KB("eq")])
                yield
                P.op("dve", lambda e, which=which: e.tensor_tensor(
                    out=eq3, in0=eq3,
                    in1=ixf[:, :, which * 16:(which + 1) * 16].unsqueeze(2).to_broadcast([128, n, 16, 16]),
                    op=ALU.mult), reads=[KB("eq"), KB("ixf")], writes=[KB("eq")])
                yield
                P.op("dve", lambda e, dst=dst: e.tensor_reduce(out=dst, in_=eq2, axis=AX.X, op=ALU.add),
                     reads=[KB("eq")], writes=[KB("i12", which)])
                yield
            P.op("dve", lambda e: e.tensor_tensor(out=ef, in0=i1, in1=i2, op=ALU.add),
                 reads=[KB("i12", 0), KB("i12", 1)], writes=[KB("ef")])
            yield
            P.op("dve", lambda e: e.tensor_copy(out=eidx[:, t0:t0 + n, h * 16:(h + 1) * 16], in_=v3(ef)),
                 reads=[KB("ef")], writes=[("eidx", t0 + c_, h) for c_ in range(n)])
            yield
            P.op("dve", lambda e: e.tensor_scalar(out=nm, in0=bvB[:, :, 0], scalar1=-1.0, scalar2=None, op0=ALU.mult),
                 reads=ck("bv", 0), writes=[KB("nm")])
            yield
            for c_ in range(n):
                P.op("act", lambda e, c_=c_: e.activation(out=ex[:, c_, :], in_=bvB[:, c_, :], func=AF.Exp,
                                                          bias=nm[:, c_:c_ + 1], scale=1.0,
                                                          accum_out=ssum[:, c_:c_ + 1]),
                     reads=[("bv", c_, par, 0), ("bv", c_, par, 1), KB("nm")], writes=[KB("ex", c_), KB("ssum", c_)])
            yield
            P.op("dve", lambda e: e.reciprocal(out=rs, in_=ssum), reads=[KB("ssum", c_) for c_ in range(n)],
                 writes=[KB("rs")])
            yield
            P.op("dve", lambda e: e.tensor_tensor(out=gates[:, t0:t0 + n, h * 16:(h + 1) * 16], in0=ex,
                                                  in1=rs.unsqueeze(2).to_broadcast([128, n, 16]), op=ALU.mult),
                 reads=[KB("ex", c_) for c_ in range(n)] + [KB("rs")],
                 writes=[("gates", t0 + c_, h) for c_ in range(n)])
            yield

        NCH = 3
        scr = [[mk_scratch(i, par) for i in range(NCH)] for par in range(2)]
        grp = [0]
        pending_tail = [None]

        def run_interleaved(gens):
            alive = [True] * len(gens)
            while any(alive):
                for gi_, g_ in enumerate(gens):
                    if alive[gi_]:
                        try:
                            next(g_)
                        except StopIteration:
                            alive[gi_] = False

        wr2 = 0
        pump_n[0] = 2
        for h in range(8):
            qb2 = q2T[h % 2]
            for p2 in range(2):
                jw = wr2 % 2
                wr2 += 1
                load_wtile(jw, wq_t_d[l, 2 * h + p2])
                for Q in range(4):
                    bank = (2 * p2 + Q) % 2
                    P.op("pe", mmgroup([(PSv[:, bank, :], Wt[jw][:, k, :], hT[:, k, Q * 512:(Q + 1) * 512], k == 0, k == 7)
                                        for k in range(8)]), reads=[("W4", jw)], writes=[("ps", bank)])
                    P.op("act", lambda e, qb2=qb2, p2=p2, Q=Q, bank=bank: e.copy(
                        out=qb2[:, p2, Q * 512:(Q + 1) * 512], in_=PSv[:, bank, :]),
                        reads=[("ps", bank)], writes=[("q2T", h % 2, p2, Q)])
            for t0 in range(0, NT, NCH):
                par = grp[0] % 2
                grp[0] += 1
                n = min(NCH, NT - t0)
                gens = [chain(h, t0 + i, scr[par][i], qb2) for i in range(n)]
                if pending_tail[0] is not None:
                    gens.append(pending_tail[0])
                run_interleaved(gens)
                pending_tail[0] = tail(h, t0, n, par)
        run_interleaved([pending_tail[0]])
        pending_tail[0] = None
        P.fence()
        if dbg and l == dbg_layer:
            P.op("sp", dma(dbg_d["d_eidx"], YA[:, 0:4096].bitcast(I32)), reads=[], writes=["dbg_e"], dma=True)
            P.op("sp", dma(dbg_d["d_gates"], YA[:, 4096:8192].bitcast(F32)), reads=[], writes=["dbg_g"], dma=True)
            P.fence()
        pump_n[0] = 0
        bg_pump(ncast)
        ebf_keys = [("ebf", ck) for ck in range(((l + 1) * 16384) // CR)]
        first_gather = [True]
        GS = 4
        adabf = ADA[:, 0:5120].bitcast(BF16)
        UV = ([HT[:, i * 2048:(i + 1) * 2048] for i in range(8)] +
              [adabf[:, i * 2048:(i + 1) * 2048] for i in range(5)] +
              [W[:, i * 2048:(i + 1) * 2048] for i in range(2)] +
              [S[:, 2048:4096], YC[:, 6144:8192]])
        NB = len(UV)
        junk2 = YC[:, 0:1024]
        ycf = YC[:].bitcast(F32)
        aall = ycf[:, 512:640]
        t1 = ycf[:, 640:768]
        sg = ycf[:, 768:896]
        wgt = ycf[:, 896:1024]
        ga = ycf[:, 1024:1152]
        ptmp = [ycf[:, 1152:1664], ycf[:, 1664:2176]]
        dg = [S[:, i * 128:(i + 1) * 128] for i in range(8)]
        gi = 0
        ngrp = 128 // GS
        for tt in range(NT):
            pb0 = 2 * (tt % 2)
            bufs_of = {}

            def stage1(gq, tt=tt):
                nonlocal gi
                cs = slice(gq * GS, (gq + 1) * GS)
                bl = []
                for hk in range(gq * GS, (gq + 1) * GS):
                    g = gi % NB
                    gi += 1
                    bl.append(g)
                    P.op("pool", lambda e, g=g, hk=hk: e.indirect_dma_start(
                        out=UV[g], out_offset=None, in_=ebf_d,
                        in_offset=bass.IndirectOffsetOnAxis(ap=eidx[:, tt, hk:hk + 1], axis=0)),
                        reads=[("eidx", tt, hk // 16)] + (ebf_keys if first_gather[0] else []),
                        writes=[("UV", g)], dma=True)
                    first_gather[0] = False
                    P.op("dve", lambda e, g=g, hk=hk: e.scalar_tensor_tensor(
                        out=junk2, in0=UV[g][:, 0:1024], scalar=1.0, in1=H2[:, tt, :], op0=ALU.mult, op1=ALU.mult,
                        accum_out=aall[:, hk:hk + 1]),
                        reads=[("UV", g), ("h2", tt)], writes=["junk2", ("a", hk)])
                bufs_of[gq] = bl
                ak = [("a", hk) for hk in range(gq * GS, (gq + 1) * GS)]
                P.op("dve", lambda e: e.tensor_tensor(out=t1[:, cs], in0=aall[:, cs], in1=aall[:, cs], op=ALU.mult),
                     reads=ak, writes=[("t1", gq)])
                P.op("dve", lambda e: e.tensor_scalar(out=t1[:, cs], in0=t1[:, cs], scalar1=0.044715, scalar2=1.0,
                                                      op0=ALU.mult, op1=ALU.add), reads=[("t1", gq)], writes=[("t1", gq)])
                P.op("dve", lambda e: e.tensor_tensor(out=t1[:, cs], in0=t1[:, cs], in1=aall[:, cs], op=ALU.mult),
                     reads=[("t1", gq)] + ak, writes=[("t1", gq)])
                P.op("dve", lambda e: e.tensor_tensor(out=ga[:, cs], in0=aall[:, cs], in1=gates[:, tt, cs], op=ALU.mult),
                     reads=ak, writes=[("ga", gq)])
                P.op("act", lambda e: e.activation(out=sg[:, cs], in_=t1[:, cs], func=AF.Sigmoid,
                                                   scale=2.0 * 0.7978845608028654),
                     reads=[("t1", gq)], writes=[("sg", gq)])

            def stage2(gq, tt=tt, pb0=pb0):
                cs = slice(gq * GS, (gq + 1) * GS)
                P.op("dve", lambda e: e.tensor_tensor(out=wgt[:, cs], in0=sg[:, cs], in1=ga[:, cs], op=ALU.mult),
                     reads=[("sg", gq), ("ga", gq)], writes=[("wgt", gq)])
                for j, hk in enumerate(range(gq * GS, (gq + 1) * GS)):
                    g = bufs_of[gq][j]
                    db = hk % 8
                    P.op("act", lambda e, db=db, hk=hk: e.activation(out=dg[db], in_=ident[:], func=AF.Copy,
                                                                     scale=wgt[:, hk:hk + 1]),
                         reads=[("wgt", gq), "ident"], writes=[("dg", db)])
                    P.op("pe", mmgroup([(PSv[:, pb0, :], dg[db], UV[g][:, 1024:1536], hk == 0, hk == 127),
                                        (PSv[:, pb0 + 1, :], dg[db], UV[g][:, 1536:2048], hk == 0, hk == 127)]),
                         reads=[("dg", db), ("UV", g)], writes=[("ps", pb0), ("ps", pb0 + 1)])

            for gq in range(ngrp):
                stage1(gq)
                if gq >= 1:
                    stage2(gq - 1)
            stage2(ngrp - 1)
            for half in range(2):
                hs = slice(half * 512, (half + 1) * 512)
                P.op("dve", lambda e, half=half, hs=hs, pb0=pb0: e.tensor_tensor(out=ptmp[half], in0=PSv[:, pb0 + half, :],
                                                                                 in1=G2[:, hs], op=ALU.mult),
                     reads=[("ps", pb0 + half)], writes=[("ptmp", half)])
                P.op("pool", lambda e, half=half, hs=hs, tt=tt: e.tensor_tensor(out=Xv[:, tt, hs], in0=Xv[:, tt, hs],
                                                                                in1=ptmp[half], op=ALU.add),
                     reads=[("ptmp", half), ("x", tt)], writes=[("x", tt)])
        P.fence()
        if dbg and l == dbg_layer:
            P.op("sp", dma(dbg_d["d_x2"], X[:].rearrange("p t d -> p (t d)")), reads=[], writes=["dbg_x2"], dma=True)
            P.fence()

    if late:
        P.op("sp", dma(dbg_d["d_eidx"], YA[:, 0:4096].bitcast(I32)), reads=[], writes=["dbg_e"], dma=True)
        P.op("sp", dma(dbg_d["d_gates"], YA[:, 4096:8192].bitcast(F32)), reads=[], writes=["dbg_g"], dma=True)
    fgr = R1[:, 0:2048].bitcast(F32)
    P.op("sp", dma(fgr, fg_d), writes=["fgr"], dma=True)
    junk = YC[:, 4096:5120]
    for tt in range(NT):
        P.op("act", lambda e, tt=tt: e.activation(out=junk, in_=Xv[:, tt, :], func=AF.Square, accum_out=ss[:, tt:tt + 1]),
             reads=[("x", tt)], writes=["junk", ("ss", tt)])
    P.op("act", lambda e: e.activation(out=srt, in_=ss, func=AF.Sqrt, scale=1.0 / D, bias=1e-6),
         reads=[("ss", tt) for tt in range(NT)], writes=["srt"])
    P.op("dve", lambda e: e.reciprocal(out=rstd, in_=srt), reads=["srt"], writes=["rstd"])
    ycf = YC[:].bitcast(F32)
    obuf = [ycf[:, 0:1024], ycf[:, 1024:2048]]
    okeys = []
    for tt in range(NT):
        ob = tt % 2
        P.op("dve", lambda e, tt=tt, ob=ob: e.scalar_tensor_tensor(out=obuf[ob], in0=Xv[:, tt, :],
                                                                   scalar=rstd[:, tt:tt + 1], in1=fgr, op0=ALU.mult,
                                                                   op1=ALU.mult),
             reads=[("x", tt), "rstd", "fgr"], writes=[("obuf", ob)])
        P.op("sp", dma(out_d[tt * 128:(tt + 1) * 128, :], obuf[ob]), reads=[("obuf", ob)], writes=[("out", tt)], dma=True)
        okeys.append(("out", tt))
    P.op("sp", lambda e: e.nop(), reads=okeys + [k for k in P.state if isinstance(k, str) and k.startswith("dbg")],
         writes=["done"])
    P.emit()
    es.close()
    return nc


def host_consts():
    slopes = np.array([2.0 ** (-8.0 * (h + 1) / 8) for h in range(8)], dtype=np.float64)
    ident = np.eye(128, dtype=np.float32)
    selB = np.zeros((128, 16, 128), np.float32)
    for j in range(16):
        selB[j, j, :] = 1.0
        e = j // 8
        selB[16 + 2 * e, j, :] = 1.0
        selB[17 + 2 * e, j, :] = 1.0
    cm = np.zeros((128, 4, 512), np.float32)
    ik = np.arange(128)[:, None]
    iq = np.arange(512)[None, :]
    for j in range(4):
        cm[:, j, :] = np.where(iq >= 128 * j + ik, 0.0, NEG)
    q = np.arange(T)
    ar = np.zeros((4, 4, T), np.float32)
    for c in range(4):
        for e in range(2):
            h = 2 * c + e
            ar[c, 2 * e, :] = -slopes[h] * 128.0 * (q // 128)
            ar[c, 2 * e + 1, :] = -slopes[h] * (q % 128)
    kb = np.zeros((128, 8, 16), np.float32)
    p = np.arange(128)
    for h in range(8):
        for kt in range(16):
            kb[:, h, kt] = slopes[h] * (128.0 * kt + p)
    iota = np.tile(np.arange(16, dtype=np.float32)[None, :], (128, 1))
    return {"c_ident": ident, "c_selB": selB.reshape(128, 2048), "c_causal": cm.reshape(128, 2048),
            "c_alibi_rows": ar, "c_kb": kb.reshape(128, 128), "c_iota16": iota}


def host_inputs(inputs):
    f = lambda a: np.ascontiguousarray(np.asarray(a, dtype=np.float32))
    shared = {
        "w_ada": f(inputs["w_ada"]),
        "b_ada_rep": f(np.broadcast_to(np.asarray(inputs["b_ada"])[:, None, :], (L, 128, 6 * D))),
        "n1g_rep": f(np.broadcast_to(np.asarray(inputs["norm1_g"])[:, None, :], (L, 128, D))),
        "n2g_rep": f(np.broadcast_to(np.asarray(inputs["norm2_g"])[:, None, :], (L, 128, D))),
        "fg_rep": f(np.broadcast_to(np.asarray(inputs["final_g"])[None, :], (128, D))),
        "w_in": f(inputs["w_in"]),
        "w_in_t": f(np.asarray(inputs["w_in"]).reshape(L, 8, 128, 40, 128).transpose(0, 3, 2, 1, 4).reshape(L, 40, 128, 1024)),
        "w_query_t": f(np.asarray(inputs["w_query"]).reshape(L, 8, 128, 16, 128).transpose(0, 3, 2, 1, 4)
                       .reshape(L, 16, 128, 1024)),
        "conv_wT": f(np.asarray(inputs["conv_w"]).transpose(0, 2, 1).reshape(L, 4, 128, 3).transpose(0, 2, 1, 3)
                     .reshape(L, 128, 12)),
        "w_attn_proj": f(inputs["w_attn_proj"]),
        "w_conv_proj": f(inputs["w_conv_proj"]),
        "w_out": f(inputs["w_out"]),
        "w_query": f(inputs["w_query"]),
        "keysT": f(np.asarray(inputs["sub_keys"]).transpose(0, 4, 1, 2, 3).reshape(L, 128, 2048)),
    }
    shared["experts_all"] = f(np.concatenate([np.asarray(inputs["expert_u"]).reshape(L * 16384, D),
                                               np.asarray(inputs["expert_v"]).reshape(L * 16384, D)], axis=1))
    shared.update(host_consts())
    x = np.asarray(inputs["x"], dtype=np.float32)
    c = np.asarray(inputs["c"], dtype=np.float32)
    maps = []
    for b in range(x.shape[0]):
        m = dict(shared)
        m["x"] = np.ascontiguousarray(x[b])
        m["crep"] = f(np.broadcast_to(c[b][:, None], (D, 128)))
        maps.append(m)
    return maps


_NC_CACHE = {}


def kernel(**inputs):
    maps = host_inputs(inputs)
    if "nc" not in _NC_CACHE:
        _NC_CACHE["nc"] = build()
    nc = _NC_CACHE["nc"]
    res = run_bass_kernel_spmd(nc, maps, core_ids=list(range(8)))
    out = np.stack([np.asarray(r["out"], dtype=np.float32) for r in res.results], axis=0)
    return out
```

```python
import numpy as np
from contextlib import ExitStack
import concourse.bass as bass
import concourse.mybir as mybir
from concourse.bass_utils import run_bass_kernel_spmd

F32 = mybir.dt.float32
BF16 = mybir.dt.bfloat16
I32 = mybir.dt.int32
U32 = mybir.dt.uint32
AF = mybir.ActivationFunctionType
ALU = mybir.AluOpType
AX = mybir.AxisListType

L = 2
T = 2048
D = 1024
NT = 16
NEG = -30000.0


class Prog:
    ENGS = ["pe", "act", "dve", "pool", "sp"]
    EPOCH = 30000

    def __init__(self, nc, n_dma_sems=32):
        self.nc = nc
        self.streams = {e: [] for e in self.ENGS}
        self.tick = {e: 0 for e in self.ENGS}
        self.ticksems = {e: [] for e in self.ENGS}
        self.dsems = [nc.alloc_semaphore(name=f"dq{i}") for i in range(n_dma_sems)]
        self.dtarget = [0] * n_dma_sems
        self.dnext = 0
        self.hsems = [nc.alloc_semaphore(name=f"hq{i}") for i in range(16)]
        self.htarget = [0] * 16
        self.hnext = 0
        self.state = {}
        self.seen = {e: {} for e in self.ENGS}
        self.semname = {}
        self.bsems = [nc.alloc_semaphore(name=f"bg{i}") for i in range(16)]
        self.btarget = [0] * 16
        self.bnext = 0
        self.bg_keys = set()

    def _ticket(self, eng):
        self.tick[eng] += 1
        ep = (self.tick[eng] - 1) // self.EPOCH
        while len(self.ticksems[eng]) <= ep:
            self.ticksems[eng].append(self.nc.alloc_semaphore(name=f"tk_{eng}_{len(self.ticksems[eng])}"))
        return (("t", eng, ep), self.tick[eng] - ep * self.EPOCH)

    def _sem(self, key):
        if key[0] == "t":
            return self.ticksems[key[1]][key[2]]
        if key[0] == "b":
            return self.bsems[key[1]]
        if key[0] == "h":
            return self.hsems[key[1]]
        return self.dsems[key[1]]

    def op(self, eng, fn, reads=(), writes=(), dma=False, bg=False):
        deps = {}

        def add(tk, own_ok):
            if tk is None:
                return
            key, val = tk
            if key[0] == "t" and key[1] == eng:
                if eng == "pe" or not own_ok:
                    return
            if deps.get(key, 0) < val:
                deps[key] = val

        for k in reads:
            st = self.state.get(k)
            if st:
                add(st["w"], True)
        for k in writes:
            st = self.state.get(k)
            if st:
                add(st["w"], True)
                for r in st["r"]:
                    add(r, True)
        if dma and bg:
            si = self.bnext
            self.bnext = (self.bnext + 1) % len(self.bsems)
            if self.btarget[si] > 0:
                add((("b", si), self.btarget[si]), True)
            self.bg_keys.update(writes)
        elif dma and eng == "sp":
            si = self.hnext
            self.hnext = (self.hnext + 1) % len(self.hsems)
            if self.htarget[si] > 0:
                add((("h", si), self.htarget[si]), True)
        elif dma:
            si = self.dnext
            self.dnext = (self.dnext + 1) % len(self.dsems)
            if self.dtarget[si] > 0:
                add((("d", si), self.dtarget[si]), True)
        waits = []
        for key, val in deps.items():
            if self.seen[eng].get(key, 0) >= val:
                continue
            self.seen[eng][key] = val
            waits.append((key, val))
        if dma and bg:
            self.btarget[si] += 16
            tk = (("b", si), self.btarget[si])
            inc = (("b", si), 16)
        elif dma and eng == "sp":
            self.htarget[si] += 16
            tk = (("h", si), self.htarget[si])
            inc = (("h", si), 16)
        elif dma:
            self.dtarget[si] += 16
            tk = (("d", si), self.dtarget[si])
            inc = (("d", si), 16)
        else:
            tk = self._ticket(eng)
            inc = (tk[0], 1)
        for k in reads:
            self.state.setdefault(k, {"w": None, "r": []})["r"].append(tk)
        for k in writes:
            st = self.state.setdefault(k, {"w": None, "r": []})
            st["w"] = tk
            st["r"] = []
        self.streams[eng].append((waits, fn, inc))
        return tk

    def fence(self):
        tks = []
        for e in self.ENGS:
            if self.tick[e] > 0:
                ep = (self.tick[e] - 1) // self.EPOCH
                tks.append((("t", e, ep), self.tick[e] - ep * self.EPOCH))
        for si, tv in enumerate(self.dtarget):
            if tv > 0:
                tks.append((("d", si), tv))
        for si, tv in enumerate(self.htarget):
            if tv > 0:
                tks.append((("h", si), tv))
        for e in self.ENGS:
            waits = []
            for key, val in tks:
                if key[0] == "t" and key[1] == e:
                    continue
                if self.seen[e].get(key, 0) >= val:
                    continue
                self.seen[e][key] = val
                waits.append((key, val))
            if waits:
                self.streams[e].append((waits, None, None))
        self.state = {k: v for k, v in self.state.items() if k in self.bg_keys}

    def emit(self):
        nc = self.nc
        with nc.Block() as block:
            def body(ename):
                def run(eng):
                    for waits, fn, inc in self.streams[ename]:
                        for key, val in waits:
                            eng.wait_ge(self._sem(key), val)
                        if fn is None:
                            continue
                        ins = fn(eng)
                        ins.then_inc(self._sem(inc[0]), inc[1])
                return run
            block.tensor(body("pe"))
            block.scalar(body("act"))
            block.vector(body("dve"))
            block.gpsimd(body("pool"))
            block.sync(body("sp"))


def mmgroup(items):
    def fn(e):
        ins = None
        for (o, l, r, s, t) in items:
            ins = e.matmul(o, l, r, start=s, stop=t)
        return ins
    return fn


def trgroup(items):
    def fn(e):
        ins = None
        for (o, i, idn) in items:
            ins = e.transpose(o, i, idn)
        return ins
    return fn


def dma(out, in_):
    return lambda e: e.dma_start(out=out, in_=in_)


def build(nlayers=L, dbg=False, dbg_layer=0, late=False):
    nc = bass.Bass("TRN2", target_bir_lowering=False)
    es = ExitStack()

    def din(name, shape, dt=F32):
        return nc.dram_tensor(name, list(shape), dt, kind="ExternalInput").ap()

    x_d = din("x", [T, D])
    crep_d = din("crep", [D, 128])
    w_ada_d = din("w_ada", [L, D, 6 * D])
    b_ada_d = din("b_ada_rep", [L, 128, 6 * D])
    n1g_d = din("n1g_rep", [L, 128, D])
    n2g_d = din("n2g_rep", [L, 128, D])
    fg_d = din("fg_rep", [128, D])
    w_in_d = din("w_in", [L, D, 5120])
    convw_d = din("conv_wT", [L, 128, 12])
    wap_d = din("w_attn_proj", [L, 512, D])
    wcp_d = din("w_conv_proj", [L, 512, D])
    wout_d = din("w_out", [L, D, D])
    wq_d = din("w_query", [L, D, 2048])
    keysT_d = din("keysT", [L, 128, 2048])
    eall_d = din("experts_all", [L * 16384, 2 * D])
    ebf_d = nc.dram_tensor("ebf", [L * 16384, 2 * D], BF16, kind="Internal").ap()
    ident_d = din("c_ident", [128, 128])
    selB_d = din("c_selB", [128, 2048])
    cm_d = din("c_causal", [128, 2048])
    ar_d = din("c_alibi_rows", [4, 4, T])
    kb_d = din("c_kb", [128, 128])
    iota_d = din("c_iota16", [128, 16])
    out_d = nc.dram_tensor("out", [T, D], F32, kind="ExternalOutput").ap()
    dbg_d = {}
    if late:
        dbg_d["d_eidx"] = nc.dram_tensor("d_eidx", [128, 2048], I32, kind="ExternalOutput").ap()
        dbg_d["d_gates"] = nc.dram_tensor("d_gates", [128, 2048], F32, kind="ExternalOutput").ap()
    if dbg:
        for nm, shp, dt in [("d_hT", [128, 16384], BF16), ("d_yaT", [128, 8192], BF16), ("d_ycT", [128, 8192], BF16),
                            ("d_x1", [128, 16384], F32), ("d_eidx", [128, 2048], I32), ("d_gates", [128, 2048], F32),
                            ("d_x2", [128, 16384], F32), ("d_x0", [128, 16384], F32), ("d_ada", [128, 6144], F32), ("d_v", [128, 8192], BF16)]:
            dbg_d[nm] = nc.dram_tensor(nm, shp, dt, kind="ExternalOutput").ap()

    def sb(name, shape, dt):
        return es.enter_context(nc.sbuf_tensor(name, list(shape), dt))

    X = sb("X", [128, NT, D], F32)
    ADA = sb("ADA", [128, 6 * D], F32)
    HT = sb("HT", [128, 16384], BF16)
    R1 = sb("R1", [128, 16384], BF16)
    YA = sb("YA", [128, 8192], BF16)
    YC = sb("YC", [128, 8192], BF16)
    S = sb("S", [128, 4096], BF16)
    W = sb("W", [128, 4096], BF16)
    ident = sb("ident", [128, 128], BF16)
    onesb = sb("onesb", [128, 128], BF16)
    kb = sb("kb", [128, 128], F32)
    iota16 = sb("iota16", [128, 16], F32)
    small = sb("small", [128, 256], F32)
    cw = sb("cw", [128, 12], F32)
    smallR = sb("smallR", [128, 2 * 3 * 96], F32)
    tails = sb("tails", [128, 512], F32)
    iotx = sb("iotx", [128, 16], F32)
    PS = es.enter_context(nc.psum_tensor("PS", [128, 8, 512], F32))

    P = Prog(nc)
    Xv = X[:]
    ADAv = ADA[:]
    hT = HT[:].rearrange("p (c t) -> p c t", c=8)
    PSv = PS[:]

    def psbf(bank):
        return PSv[:, bank, :].bitcast(BF16)

    ss = small[:, 0:16]
    srt = small[:, 16:32]
    rstd = small[:, 32:48]

    P.op("pool", dma(ident[:], ident_d), writes=["ident"], dma=True)
    P.op("sp", dma(kb[:], kb_d), writes=["kb"], dma=True)
    P.op("sp", dma(iota16[:], iota_d), writes=["iota16"], dma=True)
    P.op("pool", lambda e: e.memset(onesb[:], 1.0), writes=["onesb"])
    P.op("dve", lambda e: e.tensor_scalar(out=iotx[:], in0=iota16[:], scalar1=16.0, scalar2=None, op0=ALU.mult),
         reads=["iota16"], writes=["iotx"])
    for tt in range(NT):
        P.op("sp", dma(Xv[:, tt, :], x_d[tt * 128:(tt + 1) * 128, :]), writes=[("x", tt)], dma=True)

    CR = 512
    ncast = (nlayers * 16384) // CR
    cast_i = [0]

    def bg_pump(n=1):
        for _ in range(n):
            ck = cast_i[0]
            if ck >= ncast:
                return
            cast_i[0] += 1
            P.op("pool", dma(ebf_d[ck * CR:(ck + 1) * CR, :], eall_d[ck * CR:(ck + 1) * CR, :]),
                 writes=[("ebf", ck)], dma=True, bg=True)

    pump_n = [0]

    Wt = [W[:, j * 1024:(j + 1) * 1024].rearrange("p (k n) -> p k n", k=8) for j in range(4)]
    Wwhole = W[:].rearrange("p (k n) -> p k n", k=8)
    wkeys_all = [("W4", j) for j in range(4)]
    wrot = [0]

    def next_w():
        j = wrot[0] % 4
        wrot[0] += 1
        return j

    def load_wtile(j, src):
        P.op("pool", dma(Wt[j], src.rearrange("(k p) n -> p k n", p=128)), writes=[("W4", j)], dma=True)
        bg_pump(pump_n[0])

    def norm_phase(A_ap, B_ap, hrow_of, keep_rows):
        ycf = YC[:].bitcast(F32)
        tmpn = [ycf[:, 0:1024], ycf[:, 1024:2048]]
        junk = YC[:, 4096:5120]
        for tt in range(NT):
            P.op("act", lambda e, tt=tt: e.activation(out=junk, in_=Xv[:, tt, :], func=AF.Square,
                                                     accum_out=ss[:, tt:tt + 1]),
                 reads=[("x", tt)], writes=["junk", ("ss", tt)])
        P.op("act", lambda e: e.activation(out=srt, in_=ss, func=AF.Sqrt, scale=1.0 / D, bias=1e-6),
             reads=[("ss", tt) for tt in range(NT)], writes=["srt"])
        P.op("dve", lambda e: e.reciprocal(out=rstd, in_=srt), reads=["srt"], writes=["rstd"])
        for tt in range(NT):
            tb = tt % 2
            hr = hrow_of(tt)
            P.op("dve", lambda e, tt=tt, tb=tb: e.scalar_tensor_tensor(
                out=tmpn[tb], in0=Xv[:, tt, :], scalar=rstd[:, tt:tt + 1], in1=A_ap, op0=ALU.mult, op1=ALU.mult),
                reads=[("x", tt), "rstd", "adaA"], writes=[("tmpn", tb)])
            P.op("pool", lambda e, tb=tb, hr=hr: e.tensor_tensor(out=hr[0], in0=tmpn[tb], in1=B_ap, op=ALU.add),
                 reads=[("tmpn", tb), "adaB"], writes=[hr[1]])
            bank = tt % 2
            pb = psbf(bank)
            P.op("pe", trgroup([(pb[:, c * 128:(c + 1) * 128], hr[0][:, c * 128:(c + 1) * 128], ident[:])
                                for c in range(8)]),
                 reads=[hr[1], "ident"], writes=[("ps", bank)])
            P.op("act", lambda e, tt=tt, pb=pb: e.copy(out=hT[:, :, tt * 128:(tt + 1) * 128],
                                                      in_=pb.rearrange("p (c t) -> p c t", c=8)),
                 reads=[("ps", bank)], writes=[("hT", tt)])

    for l in range(nlayers):
        P.fence()
        if dbg and l == dbg_layer:
            P.op("sp", dma(dbg_d["d_x0"], X[:].rearrange("p t d -> p (t d)")), reads=[], writes=["dbg_x0"], dma=True)
            P.fence()
        wst = HT[:].bitcast(F32).rearrange("p (b k n) -> p b k n", b=2, k=8)
        crep = R1[:, 0:2048].bitcast(F32).rearrange("p (k m) -> p k m", k=8)
        brep = R1[:, 2048:4096].bitcast(F32).rearrange("p (b n) -> p b n", b=2)
        grep = R1[:, 4096:8192].bitcast(F32).rearrange("p (b n) -> p b n", b=2)
        P.op("sp", dma(crep, crep_d.rearrange("(k p) m -> p k m", p=128)), writes=["crep"], dma=True)
        P.op("sp", dma(grep[:, 0, :], n1g_d[l]), writes=[("grep", 0)], dma=True)
        P.op("sp", dma(grep[:, 1, :], n2g_d[l]), writes=[("grep", 1)], dma=True)
        P.op("sp", dma(cw[:], convw_d[l]), writes=["cw"], dma=True)
        wada_v = w_ada_d[l].rearrange("(k p) n -> p k n", p=128)
        for nt in range(12):
            b = nt % 2
            P.op("sp", dma(wst[:, b], wada_v[:, :, nt * 512:(nt + 1) * 512]), writes=[("wst", b)], dma=True)
            P.op("sp", dma(brep[:, b, :], b_ada_d[l][:, nt * 512:(nt + 1) * 512]), writes=[("brep", b)], dma=True)
            P.op("pe", mmgroup([(PSv[:, b, :], crep[:, k, :], wst[:, b, k, :], k == 0, k == 7) for k in range(8)]),
                 reads=[("wst", b), "crep"], writes=[("ps", b)])
            P.op("dve", lambda e, b=b, nt=nt: e.tensor_tensor(out=ADAv[:, nt * 512:(nt + 1) * 512], in0=PSv[:, b, :],
                                                              in1=brep[:, b, :], op=ALU.add),
                 reads=[("ps", b), ("brep", b)], writes=[("ada", nt)])
        P.op("dve", lambda e: e.scalar_tensor_tensor(out=ADAv[:, 1024:2048], in0=ADAv[:, 1024:2048], scalar=1.0,
                                                     in1=grep[:, 0, :], op0=ALU.add, op1=ALU.mult),
             reads=[("ada", 2), ("ada", 3), ("grep", 0)], writes=[("ada", 2), ("ada", 3)])
        P.op("dve", lambda e: e.scalar_tensor_tensor(out=ADAv[:, 4096:5120], in0=ADAv[:, 4096:5120], scalar=1.0,
                                                     in1=grep[:, 1, :], op0=ALU.add, op1=ALU.mult),
             reads=[("ada", 8), ("ada", 9), ("grep", 1)], writes=[("ada", 8), ("ada", 9)])
        if dbg and l == dbg_layer:
            P.op("sp", dma(dbg_d["d_ada"], ADAv), reads=[("ada", i) for i in range(12)], writes=["dbg_ada"], dma=True)
        P.fence()
        SH1, A1, G1 = ADAv[:, 0:1024], ADAv[:, 1024:2048], ADAv[:, 2048:3072]
        SH2, A2, G2 = ADAv[:, 3072:4096], ADAv[:, 4096:5120], ADAv[:, 5120:6144]

        hrows = [YC[:, 5120:6144], YC[:, 6144:7168]]
        norm_phase(A1, SH1, lambda tt: (hrows[tt % 2], ("hrow", tt % 2)), False)
        P.fence()
        if dbg and l == dbg_layer:
            P.op("sp", dma(dbg_d["d_hT"], HT[:]), reads=[("hT", tt) for tt in range(NT)], writes=["dbg_hT"], dma=True)
        win = w_in_d[l]
        v_sb = R1[:, 0:8192].rearrange("p (t n) -> p t n", t=16)
        qslots = R1[:, 8192:16384].rearrange("p (s t) -> p s t", s=4)
        qz = [qslots[:, 1, :], qslots[:, 2, :]]
        P.op("pool", lambda e: e.memset(qz[0][64:128, :], 0.0), writes=[("qzero", 0)])
        P.op("pool", lambda e: e.memset(qz[1][0:64, :], 0.0), writes=[("qzero", 1)])
        P.op("pool", dma(Wwhole, win[:, 1024:1536].rearrange("(k p) n -> p k n", p=128)), writes=wkeys_all, dma=True)
        for tt in range(NT):
            bank = tt % 2
            P.op("pe", mmgroup([(PSv[:, bank, :], hT[:, k, tt * 128:(tt + 1) * 128], Wwhole[:, k, :], k == 0, k == 7)
                                for k in range(8)]),
                 reads=[("hT", tt)] + wkeys_all, writes=[("ps", bank)])
            P.op("act", lambda e, tt=tt, bank=bank: e.copy(out=v_sb[:, tt, :], in_=PSv[:, bank, :]),
                 reads=[("ps", bank)], writes=[("v", tt)])
        if dbg and l == dbg_layer:
            P.op("sp", dma(dbg_d["d_v"], R1[:, 0:8192]), reads=[("v", tt) for tt in range(NT)], writes=["dbg_v"], dma=True)
        Rc = [YC[:, 0:2048], YC[:, 2048:4096]]
        selB = YC[:, 4096:6144].rearrange("p (j m) -> p j m", j=16)
        CM = YC[:, 6144:8192].rearrange("p (j q) -> p j q", j=4)
        P.op("pool", dma(YC[:, 4096:6144], selB_d), writes=["selB"], dma=True)
        P.op("pool", dma(YC[:, 6144:8192], cm_d), writes=["CM"], dma=True)
        PT = [S[:, i * 512:(i + 1) * 512] for i in range(3)]
        rcp = S[:, 2048:3072].bitcast(F32)
        gsb = small[:, 64:80]
        m8 = small[:, 80:96]
        mb32 = small[:, 96:112]
        km32 = small[:, 112:120]
        mbb = S[:, 3072:3088]
        kmT = S[:, 3104:3112]
        yaT = YA[:].rearrange("p (c t) -> p c t", c=4)
        sbi = [0]
        pti = [0]
        pump_n[0] = 0
        for c in range(4):
            cb = c % 2
            kT = qslots[:, 0, :]
            jq = next_w()
            load_wtile(jq, win[:, c * 128:(c + 1) * 128])
            jk = next_w()
            load_wtile(jk, win[:, 512 + c * 128:512 + (c + 1) * 128])
            if l == 0:
                bg_pump(8)
            P.op("pool", lambda e, cb=cb: e.memset(Rc[cb], 0.0),
                 writes=[("Rc", cb, tt) for tt in range(NT)] + [("RcA", cb)])
            P.op("pool", dma(Rc[cb][16:20, :], ar_d[c]), writes=[("RcA", cb)], dma=True)
            for Q in range(4):
                P.op("pe", mmgroup([(PSv[:, 6, :], Wt[jk][:, k, :], hT[:, k, Q * 512:(Q + 1) * 512], k == 0, k == 7)
                                    for k in range(8)]),
                     reads=[("W4", jk)] + [("hT", 4 * Q + i) for i in range(4)], writes=[("ps", 6)])
                P.op("act", lambda e, Q=Q, kT=kT: e.copy(out=kT[:, Q * 512:(Q + 1) * 512], in_=PSv[:, 6, :]),
                     reads=[("ps", 6)], writes=[("kT", 0, Q)])
                P.op("pe", mmgroup([(PSv[:, 7, :], Wt[jq][:, k, :], hT[:, k, Q * 512:(Q + 1) * 512], k == 0, k == 7)
                                    for k in range(8)]),
                     reads=[("W4", jq)] + [("hT", 4 * Q + i) for i in range(4)], writes=[("ps", 7)])
                for e8 in range(2):
                    P.op("act", lambda e, Q=Q, e8=e8: e.mul(out=qz[e8][64 * e8:64 * e8 + 64, Q * 512:(Q + 1) * 512],
                                                            in_=PSv[64 * e8:64 * e8 + 64, 7, :], mul=0.125),
                         reads=[("ps", 7)], writes=[("qT", 0, Q, e8)])
            P.op("dve", lambda e, kT=kT: e.tensor_reduce(out=km32, in_=kT.rearrange("p (n s) -> p n s", n=8),
                                                         axis=AX.X, op=ALU.add),
                 reads=[("kT", 0, Q) for Q in range(4)], writes=["km32"])
            P.op("dve", lambda e: e.tensor_scalar(out=kmT, in0=km32, scalar1=1.0 / 256, scalar2=None, op0=ALU.mult),
                 reads=["km32"], writes=["kmT"])
            for tt in range(8, NT):
                qb = tt // 2
                P.op("pe", mmgroup([(PSv[:, 5, e8 * 8:(e8 + 1) * 8],
                                     qz[e8][64 * e8:64 * e8 + 64, tt * 128:(tt + 1) * 128],
                                     kmT[64 * e8:64 * e8 + 64, :], True, True) for e8 in range(2)]),
                     reads=[("qT", 0, tt // 4, 0), ("qT", 0, tt // 4, 1), "kmT"], writes=[("ps", 5)])
                P.op("dve", lambda e: e.tensor_copy(out=gsb, in_=PSv[:, 5, 0:16]), reads=[("ps", 5)], writes=["gsb"])
                P.op("dve", lambda e, qb=qb: e.memset(gsb.rearrange("p (a n) -> p a n", a=2)[:, :, qb:8], -1e30),
                     reads=["gsb"], writes=["gsb"])
                for e8 in range(2):
                    P.op("dve", lambda e, e8=e8: e.max(out=m8[:, e8 * 8:(e8 + 1) * 8], in_=gsb[:, e8 * 8:(e8 + 1) * 8]),
                         reads=["gsb"], writes=[("m8", e8)])
                for e8 in range(2):
                    P.op("dve", lambda e, e8=e8: e.tensor_scalar(out=mb32[:, e8 * 8:(e8 + 1) * 8],
                                                                 in0=gsb[:, e8 * 8:(e8 + 1) * 8],
                                                                 scalar1=m8[:, e8 * 8 + 2:e8 * 8 + 3], scalar2=None,
                                                                 op0=ALU.is_ge),
                         reads=["gsb", ("m8", e8)], writes=[("mb32", e8)])
                P.op("dve", lambda e: e.tensor_scalar(out=mbb, in0=mb32, scalar1=-1.0, scalar2=-NEG, op0=ALU.add,
                                                      op1=ALU.mult),
                     reads=[("mb32", 0), ("mb32", 1)], writes=["mbb"])
                P.op("dve", lambda e, qb=qb: e.memset(mbb.rearrange("p (a n) -> p a n", a=2)[:, :, qb:8], 0.0),
                     reads=["mbb"], writes=["mbb"])
                pb5 = psbf(5)
                P.op("pe", trgroup([(pb5[0:16, 512:640], mbb, ident[:])]), reads=["mbb", "ident"], writes=[("ps", 5)])
                P.op("act", lambda e, tt=tt, cb=cb, pb5=pb5: e.copy(out=Rc[cb][0:16, tt * 128:(tt + 1) * 128],
                                                                   in_=pb5[0:16, 512:640]),
                     reads=[("ps", 5)], writes=[("Rc", cb, tt)])
            steps = [(e8, Q, kt) for Q in range(4) for e8 in range(2) for kt in range(4 * Q + 4)]
            pend = None

            def emit_pv(st):
                e8, Q, kt, ptb = st
                nkt = 4 * Q + 4
                lo, hi = 64 * e8, 64 * e8 + 64
                P.op("pe", mmgroup([(PSv[:, 3, :], v_sb[:, kt, c * 128:(c + 1) * 128], PT[ptb], kt == 0,
                                     kt == nkt - 1),
                                    (PSv[:, 4, :], onesb[:], PT[ptb], kt == 0, kt == nkt - 1)]),
                     reads=[("PT", ptb), ("v", kt), "onesb"], writes=[("ps", 3), ("ps", 4)])
                if kt == nkt - 1:
                    P.op("dve", lambda e, lo=lo, hi=hi: e.reciprocal(out=rcp[lo:hi, :], in_=PSv[lo:hi, 4, :]),
                         reads=[("ps", 4)], writes=["rcp"])
                    P.op("dve", lambda e, lo=lo, hi=hi, Q=Q, c=c: e.tensor_tensor(
                        out=yaT[lo:hi, c, Q * 512:(Q + 1) * 512], in0=PSv[lo:hi, 3, :], in1=rcp[lo:hi, :], op=ALU.mult),
                        reads=[("ps", 3), "rcp"], writes=[("yaT", c, Q, e8)])

            for (e8, Q, kt) in steps:
                h = 2 * c + e8
                lo, hi = 64 * e8, 64 * e8 + 64
                n = kt // 2
                sbk = sbi[0] % 3
                sbi[0] += 1
                ptb = pti[0] % 3
                pti[0] += 1
                items = [(PSv[:, sbk, :], kT[:, kt * 128:(kt + 1) * 128], qz[e8][:, Q * 512:(Q + 1) * 512],
                          True, False),
                         (PSv[:, sbk, :], selB[:, e8 * 8 + n, :], Rc[cb][:, Q * 512:(Q + 1) * 512], False,
                          kt < 4 * Q)]
                if kt >= 4 * Q:
                    items.append((PSv[:, sbk, :], ident[:], CM[:, kt - 4 * Q, :], False, True))
                P.op("pe", mmgroup(items),
                     reads=[("kT", 0, kt // 4), ("qT", 0, Q, e8), ("qzero", e8), "selB", "CM", "ident", ("RcA", cb)] +
                           [("Rc", cb, 4 * Q + i) for i in range(4)],
                     writes=[("ps", sbk)])
                P.op("act", lambda e, sbk=sbk, ptb=ptb, h=h, kt=kt: e.activation(
                    out=PT[ptb], in_=PSv[:, sbk, :], func=AF.Exp, bias=kb[:, h * 16 + kt:h * 16 + kt + 1],
                    scale=1.0),
                    reads=[("ps", sbk), "kb"], writes=[("PT", ptb)])
                if pend is not None:
                    emit_pv(pend)
                pend = (e8, Q, kt, ptb)
            emit_pv(pend)
        P.fence()
        if dbg and l == dbg_layer:
            P.op("sp", dma(dbg_d["d_yaT"], YA[:]), reads=[], writes=["dbg_ya"], dma=True)
            P.fence()

        pump_n[0] = 0
        r1f = R1[:].bitcast(F32)
        zf = r1f[:, 0:2050]
        yf = r1f[:, 2304:4352]
        hcs = [r1f[:, 4608:5120], r1f[:, 5120:5632]]
        ycT = YC[:].rearrange("p (c t) -> p c t", c=4)
        P.op("dve", lambda e: e.memset(zf[:, 0:2], 0.0), writes=["z0"])
        for cc in range(4):
            jc = next_w()
            load_wtile(jc, win[:, 2048 + cc * 128:2048 + (cc + 1) * 128])
            jh = next_w()
            load_wtile(jh, win[:, 2560 + cc * 128:2560 + (cc + 1) * 128])
            jb = next_w()
            load_wtile(jb, win[:, 1536 + cc * 128:1536 + (cc + 1) * 128])
            for Q in range(4):
                hb = Q % 2
                bc_, bh_ = 4 * hb, 4 * hb + 1
                P.op("pe", mmgroup([(PSv[:, bc_, :], Wt[jc][:, k, :], hT[:, k, Q * 512:(Q + 1) * 512], k == 0, k == 7)
                                    for k in range(8)]), reads=[("W4", jc)], writes=[("ps", bc_)])
                P.op("pe", mmgroup([(PSv[:, bh_, :], Wt[jh][:, k, :], hT[:, k, Q * 512:(Q + 1) * 512], k == 0, k == 7)
                                    for k in range(8)]), reads=[("W4", jh)], writes=[("ps", bh_)])
                P.op("act", lambda e, hb=hb, bh_=bh_: e.copy(out=hcs[hb], in_=PSv[:, bh_, :]), reads=[("ps", bh_)],
                     writes=[("hcs", hb)])
                P.op("dve", lambda e, hb=hb, Q=Q, bc_=bc_: e.tensor_tensor(out=zf[:, 2 + Q * 512:2 + (Q + 1) * 512],
                                                                          in0=PSv[:, bc_, :], in1=hcs[hb], op=ALU.mult),
                     reads=[("ps", bc_), ("hcs", hb)], writes=[("z", Q)])
            zr = [("z", Q) for Q in range(4)] + ["z0"]
            P.op("dve", lambda e, cc=cc: e.tensor_scalar(out=yf, in0=zf[:, 2:2050], scalar1=cw[:, cc * 3 + 2:cc * 3 + 3],
                                                         scalar2=None, op0=ALU.mult), reads=zr + ["cw"], writes=["y"])
            P.op("dve", lambda e, cc=cc: e.scalar_tensor_tensor(out=yf, in0=zf[:, 1:2049],
                                                                scalar=cw[:, cc * 3 + 1:cc * 3 + 2], in1=yf,
                                                                op0=ALU.mult, op1=ALU.add), reads=zr + ["y"], writes=["y"])
            P.op("dve", lambda e, cc=cc: e.scalar_tensor_tensor(out=yf, in0=zf[:, 0:2048],
                                                                scalar=cw[:, cc * 3:cc * 3 + 1], in1=yf,
                                                                op0=ALU.mult, op1=ALU.add), reads=zr + ["y"], writes=["y"])
            for Q in range(4):
                bank = 2 + Q % 2
                P.op("pe", mmgroup([(PSv[:, bank, :], Wt[jb][:, k, :], hT[:, k, Q * 512:(Q + 1) * 512], k == 0, k == 7)
                                    for k in range(8)]), reads=[("W4", jb)], writes=[("ps", bank)])
                P.op("dve", lambda e, bank=bank, cc=cc, Q=Q: e.tensor_tensor(
                    out=ycT[:, cc, Q * 512:(Q + 1) * 512], in0=PSv[:, bank, :], in1=yf[:, Q * 512:(Q + 1) * 512],
                    op=ALU.mult), reads=[("ps", bank), "y"], writes=[("ycT", cc, Q)])
        P.fence()
        if dbg and l == dbg_layer:
            P.op("sp", dma(dbg_d["d_ycT"], YC[:]), reads=[], writes=["dbg_yc"], dma=True)
            P.fence()

        mT = R1[:].rearrange("p (c t) -> p c t", c=8)
        sf = S[:].bitcast(F32)
        sa = sf[:, 0:512]
        sc = sf[:, 512:1024]
        WAC = [S[:, 2048 + i * 512:2048 + (i + 1) * 512].rearrange("p (k n) -> p k n", k=4) for i in range(4)]
        wap_v = wap_d[l].rearrange("(k p) n -> p k n", p=128)
        wcp_v = wcp_d[l].rearrange("(k p) n -> p k n", p=128)
        it = 0
        for dc in range(8):
            jga = next_w()
            load_wtile(jga, win[:, 3072 + dc * 128:3072 + (dc + 1) * 128])
            jgc = next_w()
            load_wtile(jgc, win[:, 4096 + dc * 128:4096 + (dc + 1) * 128])
            wa = WAC[(dc % 2) * 2]
            wc = WAC[(dc % 2) * 2 + 1]
            P.op("pool", dma(wa, wap_v[:, :, dc * 128:(dc + 1) * 128]), writes=[("WAC", (dc % 2) * 2)], dma=True)
            P.op("pool", dma(wc, wcp_v[:, :, dc * 128:(dc + 1) * 128]), writes=[("WAC", (dc % 2) * 2 + 1)], dma=True)
            for Q in range(4):
                b0 = 4 * (it % 2)
                it += 1
                qs = slice(Q * 512, (Q + 1) * 512)
                P.op("pe", mmgroup([(PSv[:, b0, :], wa[:, k, :], yaT[:, k, qs], k == 0, k == 3) for k in range(4)]),
                     reads=[("WAC", (dc % 2) * 2)], writes=[("ps", b0)])
                P.op("pe", mmgroup([(PSv[:, b0 + 1, :], Wt[jga][:, k, :], hT[:, k, qs], k == 0, k == 7)
                                    for k in range(8)]), reads=[("W4", jga)], writes=[("ps", b0 + 1)])
                P.op("pe", mmgroup([(PSv[:, b0 + 2, :], wc[:, k, :], ycT[:, k, qs], k == 0, k == 3) for k in range(4)]),
                     reads=[("WAC", (dc % 2) * 2 + 1)], writes=[("ps", b0 + 2)])
                P.op("pe", mmgroup([(PSv[:, b0 + 3, :], Wt[jgc][:, k, :], hT[:, k, qs], k == 0, k == 7)
                                    for k in range(8)]), reads=[("W4", jgc)], writes=[("ps", b0 + 3)])
                P.op("act", lambda e, b0=b0: e.activation(out=sa, in_=PSv[:, b0 + 1, :], func=AF.Sigmoid),
                     reads=[("ps", b0 + 1)], writes=["sa"])
                P.op("act", lambda e, b0=b0: e.activation(out=sc, in_=PSv[:, b0 + 3, :], func=AF.Sigmoid),
                     reads=[("ps", b0 + 3)], writes=["sc"])
                P.op("dve", lambda e, b0=b0: e.tensor_tensor(out=sa, in0=sa, in1=PSv[:, b0, :], op=ALU.mult),
                     reads=["sa", ("ps", b0)], writes=["sa"])
                P.op("dve", lambda e, b0=b0: e.tensor_tensor(out=sc, in0=sc, in1=PSv[:, b0 + 2, :], op=ALU.mult),
                     reads=["sc", ("ps", b0 + 2)], writes=["sc"])
                P.op("pool", lambda e, dc=dc, qs=qs: e.tensor_tensor(out=mT[:, dc, qs], in0=sa, in1=sc, op=ALU.add),
                     reads=["sa", "sc"], writes=[("mT", dc, Q)])
        P.fence()
        otmp = [sf[:, 0:512], sf[:, 512:1024]]
        wout_v = wout_d[l].rearrange("(k p) n -> p k n", p=128)
        for half in range(2):
            hs = slice(half * 512, (half + 1) * 512)
            P.op("pool", dma(Wwhole, wout_v[:, :, hs]), writes=wkeys_all, dma=True)
            for tt in range(NT):
                bank = tt % 2
                P.op("pe", mmgroup([(PSv[:, bank, :], mT[:, k, tt * 128:(tt + 1) * 128], Wwhole[:, k, :], k == 0, k == 7)
                                    for k in range(8)]), reads=wkeys_all, writes=[("ps", bank)])
                P.op("dve", lambda e, bank=bank, hs=hs: e.tensor_tensor(out=otmp[bank], in0=PSv[:, bank, :],
                                                                        in1=G1[:, hs], op=ALU.mult),
                     reads=[("ps", bank)], writes=[("otmp", bank)])
                P.op("pool", lambda e, bank=bank, hs=hs, tt=tt: e.tensor_tensor(out=Xv[:, tt, hs], in0=Xv[:, tt, hs],
                                                                                in1=otmp[bank], op=ALU.add),
                     reads=[("otmp", bank), ("x", tt)], writes=[("x", tt)])
        P.fence()
        if dbg and l == dbg_layer:
            P.op("sp", dma(dbg_d["d_x1"], X[:].rearrange("p t d -> p (t d)")), reads=[], writes=["dbg_x1"], dma=True)
            P.fence()

        H2 = R1[:].rearrange("p (t d) -> p t d", t=16)
        norm_phase(A2, SH2, lambda tt: (H2[:, tt, :], ("h2", tt)), True)
        P.fence()
        keysT = S[:, 0:2048].rearrange("p (j n) -> p j n", j=16)
        P.op("pool", dma(S[:, 0:2048], keysT_d[l]), writes=["keysT"], dma=True)
        q2T = [YC[:, 0:4096].rearrange("p (w t) -> p w t", w=2), YC[:, 4096:8192].rearrange("p (w t) -> p w t", w=2)]
        eidx = YA[:, 0:4096].bitcast(I32).rearrange("p (t k) -> p t k", t=16)
        gates = YA[:, 4096:8192].bitcast(F32).rearrange("p (t k) -> p t k", t=16)
        wqv = wq_d[l]

        smR = smallR[:].rearrange("p (g c w) -> p g c w", g=2, c=3)

        def mk_scratch(cid, par):
            base = [S[:, 2048:4096].bitcast(F32), W[:, 2048:4096].bitcast(F32), ADAv[:, 3072:4096]][cid]
            sm = smR[:, par, cid, :]
            return dict(scs=base[:, 0:256], scw=base[:, 256:512], cand=base[:, 512:768], candw=base[:, 768:1024],
                        vv=sm[:, 0:32], ixu=sm[:, 32:64].bitcast(U32), bv=sm[:, 64:80],
                        posu=sm[:, 80:96].bitcast(U32), cid=cid, par=par)

        def chain(h, tt, sc, qb2):
            KS_ = lambda name, *a: (name, sc["cid"]) + tuple(a)
            K_ = lambda name, *a: (name, sc["cid"], sc["par"]) + tuple(a)
            scs, scw, cand, candw = sc["scs"], sc["scw"], sc["cand"], sc["candw"]
            vv, ixu, bv, posu = sc["vv"], sc["ixu"], sc["bv"], sc["posu"]
            bank = 2 + tt % 2
            ts_ = slice(tt * 128, (tt + 1) * 128)
            P.op("pe", mmgroup([(PSv[:, bank, p2 * 128:(p2 + 1) * 128], qb2[:, p2, ts_], keysT[:, 2 * h + p2, :],
                                 True, True) for p2 in range(2)]),
                 reads=[("q2T", h % 2, p2, tt // 4) for p2 in range(2)] + ["keysT"], writes=[("ps", bank)])
            P.op("act", lambda e: e.copy(out=scs, in_=PSv[:, bank, 0:256]), reads=[("ps", bank)], writes=[KS_("scs")])
            yield
            for p2 in range(2):
                sl = slice(p2 * 128, (p2 + 1) * 128)
                v0 = slice(p2 * 16, p2 * 16 + 8)
                v1 = slice(p2 * 16 + 8, p2 * 16 + 16)
                P.op("dve", lambda e, v0=v0, sl=sl: e.max(out=vv[:, v0], in_=scs[:, sl]), reads=[KS_("scs")], writes=[K_("vv", p2, 0)])
                yield
                P.op("dve", lambda e, v0=v0, sl=sl: e.max_index(out=ixu[:, v0], in_max=vv[:, v0], in_values=scs[:, sl]),
                     reads=[KS_("scs"), K_("vv", p2, 0)], writes=[K_("ix", p2, 0)])
                yield
                P.op("dve", lambda e, v0=v0, sl=sl: e.match_replace(out=scw[:, sl], in_to_replace=vv[:, v0], in_values=scs[:, sl],
                                                      imm_value=-1e30),
                     reads=[KS_("scs"), K_("vv", p2, 0)], writes=[KS_("scw", p2)])
                yield
                P.op("dve", lambda e, v1=v1, sl=sl: e.max(out=vv[:, v1], in_=scw[:, sl]), reads=[KS_("scw", p2)],
                     writes=[K_("vv", p2, 1)])
                yield
                P.op("dve", lambda e, v1=v1, sl=sl: e.max_index(out=ixu[:, v1], in_max=vv[:, v1], in_values=scw[:, sl]),
                     reads=[KS_("scw", p2), K_("vv", p2, 1)], writes=[K_("ix", p2, 1)])
                yield
            vkeys = [K_("vv", a, b) for a in range(2) for b in range(2)]
            ikeys = [K_("ix", a, b) for a in range(2) for b in range(2)]
            c3 = cand.rearrange("p (a b) -> p a b", a=16)
            P.op("dve", lambda e: e.tensor_tensor(out=c3, in0=vv[:, 0:16].unsqueeze(2).to_broadcast([128, 16, 16]),
                                                  in1=vv[:, 16:32].unsqueeze(1).to_broadcast([128, 16, 16]),
                                                  op=ALU.add), reads=vkeys, writes=[KS_("cand")])
            yield
            P.op("dve", lambda e: e.max(out=bv[:, 0:8], in_=cand), reads=[KS_("cand")], writes=[K_("bv", 0)])
            yield
            P.op("dve", lambda e: e.max_index(out=posu[:, 0:8], in_max=bv[:, 0:8], in_values=cand),
                 reads=[KS_("cand"), K_("bv", 0)], writes=[K_("pos", 0)])
            yield
            P.op("dve", lambda e: e.match_replace(out=candw, in_to_replace=bv[:, 0:8], in_values=cand,
                                                  imm_value=-1e30), reads=[KS_("cand"), K_("bv", 0)],
                 writes=[KS_("candw")])
            yield
            P.op("dve", lambda e: e.max(out=bv[:, 8:16], in_=candw), reads=[KS_("candw")], writes=[K_("bv", 1)])
            yield
            P.op("dve", lambda e: e.max_index(out=posu[:, 8:16], in_max=bv[:, 8:16], in_values=candw),
                 reads=[KS_("candw"), K_("bv", 1)], writes=[K_("pos", 1)])
            yield

        eqB = ADAv[:, 4096:4864]

        def tail(h, t0, n, par):
            KB = lambda name, *a: ("tl", name) + tuple(a)
            ck = lambda name, *a: [(name, c_, par) + tuple(a) for c_ in range(n)]
            sm = smR[:, par, 0:n, :]
            ixuB = sm[:, :, 32:64].bitcast(U32)
            bvB = sm[:, :, 64:80]
            posuB = sm[:, :, 80:96].bitcast(U32)
            m = n * 16
            posf = tails[:, 0:m]
            a16 = tails[:, 48:48 + m]
            bq = tails[:, 96:96 + m]
            i1 = tails[:, 144:144 + m]
            i2 = tails[:, 192:192 + m]
            ef = tails[:, 240:240 + m]
            ixf = tails[:, 288:288 + 2 * m].rearrange("p (c w) -> p c w", c=n)
            ex = tails[:, 384:384 + m].rearrange("p (c w) -> p c w", c=n)
            nm = tails[:, 432:432 + n]
            ssum = tails[:, 440:440 + n]
            rs = tails[:, 448:448 + n]
            eq2 = eqB[:, 0:m * 16].rearrange("p (k a) -> p k a", a=16)
            eq3 = eqB[:, 0:m * 16].rearrange("p (c k a) -> p c k a", c=n, a=16)
            v3 = lambda ap: ap.rearrange("p (c w) -> p c w", c=n)
            bc_k = lambda ap: ap.unsqueeze(2).to_broadcast([128, m, 16])
            bc_a = lambda ap: ap.unsqueeze(1).to_broadcast([128, m, 16])
            P.op("dve", lambda e: e.tensor_copy(out=v3(posf), in_=posuB), reads=ck("pos", 0) + ck("pos", 1),
                 writes=[KB("posf")])
            yield
            P.op("dve", lambda e: e.scalar_tensor_tensor(out=eq2, in0=bc_a(iota16[:]), scalar=16.0, in1=bc_k(posf),
                                                         op0=ALU.mult, op1=ALU.is_le),
                 reads=[KB("posf")], writes=[KB("eq")])
            yield
            P.op("dve", lambda e: e.tensor_reduce(out=a16, in_=eq2, axis=AX.X, op=ALU.add), reads=[KB("eq")],
                 writes=[KB("a16")])
            yield
            P.op("dve", lambda e: e.tensor_scalar(out=a16, in0=a16, scalar1=16.0, scalar2=-16.0, op0=ALU.mult,
                                                  op1=ALU.add), reads=[KB("a16")], writes=[KB("a16")])
            yield
            P.op("dve", lambda e: e.tensor_tensor(out=bq, in0=posf, in1=a16, op=ALU.subtract),
                 reads=[KB("posf"), KB("a16")], writes=[KB("bq")])
            yield
            P.op("dve", lambda e: e.tensor_copy(out=ixf, in_=ixuB),
                 reads=[k_ for a_ in range(2) for b_ in range(2) for k_ in ck("ix", a_, b_)], writes=[KB("ixf")])
            yield
            P.op("dve", lambda e, loff=float(l * 16384): e.tensor_scalar(
                out=ixf[:, :, 0:16], in0=ixf[:, :, 0:16], scalar1=128.0, scalar2=loff, op0=ALU.mult, op1=ALU.add),
                 reads=[KB("ixf")], writes=[KB("ixf")])
            yield
            for which in range(2):
                src = a16 if which == 0 else bq
                mul = 16.0 if which == 0 else 1.0
                dst = i1 if which == 0 else i2
                P.op("dve", lambda e, src=src, mul=mul: e.scalar_tensor_tensor(
                    out=eq2, in0=bc_a(iota16[:]), scalar=mul, in1=bc_k(src), op0=ALU.mult, op1=ALU.is_equal),
                    reads=[KB("a16"), KB("bq"), KB("eq")], writes=[KB("eq")])
                yield
                P.op("dve", lambda e, which=which: e.tensor_tensor(
                    out=eq3, in0=eq3,
                    in1=ixf[:, :, which * 16:(which + 1) * 16].unsqueeze(2).to_broadcast([128, n, 16, 16]),
                    op=ALU.mult), reads=[KB("eq"), KB("ixf")], writes=[KB("eq")])
                yield
                P.op("dve", lambda e, dst=dst: e.tensor_reduce(out=dst, in_=eq2, axis=AX.X, op=ALU.add),
                     reads=[KB("eq")], writes=[KB("i12", which)])
                yield
            P.op("dve", lambda e: e.tensor_tensor(out=ef, in0=i1, in1=i2, op=ALU.add),
                 reads=[KB("i12", 0), KB("i12", 1)], writes=[KB("ef")])
            yield
            P.op("dve", lambda e: e.tensor_copy(out=eidx[:, t0:t0 + n, h * 16:(h + 1) * 16], in_=v3(ef)),
                 reads=[KB("ef")], writes=[("eidx", t0 + c_, h) for c_ in range(n)])
            yield
            P.op("dve", lambda e: e.tensor_scalar(out=nm, in0=bvB[:, :, 0], scalar1=-1.0, scalar2=None, op0=ALU.mult),
                 reads=ck("bv", 0), writes=[KB("nm")])
            yield
            for c_ in range(n):
                P.op("act", lambda e, c_=c_: e.activation(out=ex[:, c_, :], in_=bvB[:, c_, :], func=AF.Exp,
                                                          bias=nm[:, c_:c_ + 1], scale=1.0,
                                                          accum_out=ssum[:, c_:c_ + 1]),
                     reads=[("bv", c_, par, 0), ("bv", c_, par, 1), KB("nm")], writes=[KB("ex", c_), KB("ssum", c_)])
            yield
            P.op("dve", lambda e: e.reciprocal(out=rs, in_=ssum), reads=[KB("ssum", c_) for c_ in range(n)],
                 writes=[KB("rs")])
            yield
            P.op("dve", lambda e: e.tensor_tensor(out=gates[:, t0:t0 + n, h * 16:(h + 1) * 16], in0=ex,
                                                  in1=rs.unsqueeze(2).to_broadcast([128, n, 16]), op=ALU.mult),
                 reads=[KB("ex", c_) for c_ in range(n)] + [KB("rs")],
                 writes=[("gates", t0 + c_, h) for c_ in range(n)])
            yield

        NCH = 3
        scr = [[mk_scratch(i, par) for i in range(NCH)] for par in range(2)]
        grp = [0]
        pending_tail = [None]

        def run_interleaved(gens):
            alive = [True] * len(gens)
            while any(alive):
                for gi_, g_ in enumerate(gens):
                    if alive[gi_]:
                        try:
                            next(g_)
                        except StopIteration:
                            alive[gi_] = False

        wr2 = 0
        pump_n[0] = 2
        for h in range(8):
            qb2 = q2T[h % 2]
            for p2 in range(2):
                jw = wr2 % 2
                wr2 += 1
                load_wtile(jw, wqv[:, (2 * h + p2) * 128:(2 * h + p2 + 1) * 128])
                for Q in range(4):
                    bank = (2 * p2 + Q) % 2
                    P.op("pe", mmgroup([(PSv[:, bank, :], Wt[jw][:, k, :], hT[:, k, Q * 512:(Q + 1) * 512], k == 0, k == 7)
                                        for k in range(8)]), reads=[("W4", jw)], writes=[("ps", bank)])
                    P.op("act", lambda e, qb2=qb2, p2=p2, Q=Q, bank=bank: e.copy(
                        out=qb2[:, p2, Q * 512:(Q + 1) * 512], in_=PSv[:, bank, :]),
                        reads=[("ps", bank)], writes=[("q2T", h % 2, p2, Q)])
            for t0 in range(0, NT, NCH):
                par = grp[0] % 2
                grp[0] += 1
                n = min(NCH, NT - t0)
                gens = [chain(h, t0 + i, scr[par][i], qb2) for i in range(n)]
                if pending_tail[0] is not None:
                    gens.append(pending_tail[0])
                run_interleaved(gens)
                pending_tail[0] = tail(h, t0, n, par)
        run_interleaved([pending_tail[0]])
        pending_tail[0] = None
        P.fence()
        if dbg and l == dbg_layer:
            P.op("sp", dma(dbg_d["d_eidx"], YA[:, 0:4096].bitcast(I32)), reads=[], writes=["dbg_e"], dma=True)
            P.op("sp", dma(dbg_d["d_gates"], YA[:, 4096:8192].bitcast(F32)), reads=[], writes=["dbg_g"], dma=True)
            P.fence()
        pump_n[0] = 0
        bg_pump(ncast)
        ebf_keys = [("ebf", ck) for ck in range(((l + 1) * 16384) // CR)]
        first_gather = [True]
        GS = 4
        adabf = ADA[:, 0:5120].bitcast(BF16)
        UV = ([HT[:, i * 2048:(i + 1) * 2048] for i in range(8)] +
              [adabf[:, i * 2048:(i + 1) * 2048] for i in range(5)] +
              [W[:, i * 2048:(i + 1) * 2048] for i in range(2)] +
              [S[:, 2048:4096], YC[:, 6144:8192]])
        NB = len(UV)
        junk2 = YC[:, 0:1024]
        ycf = YC[:].bitcast(F32)
        aall = ycf[:, 512:640]
        t1 = ycf[:, 640:768]
        sg = ycf[:, 768:896]
        wgt = ycf[:, 896:1024]
        ga = ycf[:, 1024:1152]
        ptmp = [ycf[:, 1152:1664], ycf[:, 1664:2176]]
        dg = [S[:, i * 128:(i + 1) * 128] for i in range(8)]
        gi = 0
        ngrp = 128 // GS
        for tt in range(NT):
            pb0 = 2 * (tt % 2)
            bufs_of = {}

            def stage1(gq, tt=tt):
                nonlocal gi
                cs = slice(gq * GS, (gq + 1) * GS)
                bl = []
                for hk in range(gq * GS, (gq + 1) * GS):
                    g = gi % NB
                    gi += 1
                    bl.append(g)
                    P.op("pool", lambda e, g=g, hk=hk: e.indirect_dma_start(
                        out=UV[g], out_offset=None, in_=ebf_d,
                        in_offset=bass.IndirectOffsetOnAxis(ap=eidx[:, tt, hk:hk + 1], axis=0)),
                        reads=[("eidx", tt, hk // 16)] + (ebf_keys if first_gather[0] else []),
                        writes=[("UV", g)], dma=True)
                    first_gather[0] = False
                    P.op("dve", lambda e, g=g, hk=hk: e.scalar_tensor_tensor(
                        out=junk2, in0=UV[g][:, 0:1024], scalar=1.0, in1=H2[:, tt, :], op0=ALU.mult, op1=ALU.mult,
                        accum_out=aall[:, hk:hk + 1]),
                        reads=[("UV", g), ("h2", tt)], writes=["junk2", ("a", hk)])
                bufs_of[gq] = bl
                ak = [("a", hk) for hk in range(gq * GS, (gq + 1) * GS)]
                P.op("dve", lambda e: e.scalar_tensor_tensor(out=t1[:, cs], in0=aall[:, cs], scalar=0.044715,
                                                             in1=aall[:, cs], op0=ALU.mult, op1=ALU.mult),
                     reads=ak, writes=[("t1", gq)])
                P.op("dve", lambda e: e.scalar_tensor_tensor(out=t1[:, cs], in0=t1[:, cs], scalar=1.0,
                                                             in1=aall[:, cs], op0=ALU.add, op1=ALU.mult),
                     reads=[("t1", gq)] + ak, writes=[("t1", gq)])
                P.op("dve", lambda e: e.tensor_tensor(out=ga[:, cs], in0=aall[:, cs], in1=gates[:, tt, cs], op=ALU.mult),
                     reads=ak, writes=[("ga", gq)])
                P.op("act", lambda e: e.activation(out=sg[:, cs], in_=t1[:, cs], func=AF.Sigmoid,
                                                   scale=2.0 * 0.7978845608028654),
                     reads=[("t1", gq)], writes=[("sg", gq)])

            def stage2(gq, tt=tt, pb0=pb0):
                cs = slice(gq * GS, (gq + 1) * GS)
                P.op("dve", lambda e: e.tensor_tensor(out=wgt[:, cs], in0=sg[:, cs], in1=ga[:, cs], op=ALU.mult),
                     reads=[("sg", gq), ("ga", gq)], writes=[("wgt", gq)])
                for j, hk in enumerate(range(gq * GS, (gq + 1) * GS)):
                    g = bufs_of[gq][j]
                    db = hk % 8
                    P.op("act", lambda e, db=db, hk=hk: e.activation(out=dg[db], in_=ident[:], func=AF.Copy,
                                                                     scale=wgt[:, hk:hk + 1]),
                         reads=[("wgt", gq), "ident"], writes=[("dg", db)])
                    P.op("pe", mmgroup([(PSv[:, pb0, :], dg[db], UV[g][:, 1024:1536], hk == 0, hk == 127),
                                        (PSv[:, pb0 + 1, :], dg[db], UV[g][:, 1536:2048], hk == 0, hk == 127)]),
                         reads=[("dg", db), ("UV", g)], writes=[("ps", pb0), ("ps", pb0 + 1)])

            for gq in range(ngrp):
                stage1(gq)
                if gq >= 1:
                    stage2(gq - 1)
            stage2(ngrp - 1)
            for half in range(2):
                hs = slice(half * 512, (half + 1) * 512)
                P.op("dve", lambda e, half=half, hs=hs, pb0=pb0: e.tensor_tensor(out=ptmp[half], in0=PSv[:, pb0 + half, :],
                                                                                 in1=G2[:, hs], op=ALU.mult),
                     reads=[("ps", pb0 + half)], writes=[("ptmp", half)])
                P.op("pool", lambda e, half=half, hs=hs, tt=tt: e.tensor_tensor(out=Xv[:, tt, hs], in0=Xv[:, tt, hs],
                                                                                in1=ptmp[half], op=ALU.add),
                     reads=[("ptmp", half), ("x", tt)], writes=[("x", tt)])
        P.fence()
        if dbg and l == dbg_layer:
            P.op("sp", dma(dbg_d["d_x2"], X[:].rearrange("p t d -> p (t d)")), reads=[], writes=["dbg_x2"], dma=True)
            P.fence()

    if late:
        P.op("sp", dma(dbg_d["d_eidx"], YA[:, 0:4096].bitcast(I32)), reads=[], writes=["dbg_e"], dma=True)
        P.op("sp", dma(dbg_d["d_gates"], YA[:, 4096:8192].bitcast(F32)), reads=[], writes=["dbg_g"], dma=True)
    fgr = R1[:, 0:2048].bitcast(F32)
    P.op("sp", dma(fgr, fg_d), writes=["fgr"], dma=True)
    junk = YC[:, 4096:5120]
    for tt in range(NT):
        P.op("act", lambda e, tt=tt: e.activation(out=junk, in_=Xv[:, tt, :], func=AF.Square, accum_out=ss[:, tt:tt + 1]),
             reads=[("x", tt)], writes=["junk", ("ss", tt)])
    P.op("act", lambda e: e.activation(out=srt, in_=ss, func=AF.Sqrt, scale=1.0 / D, bias=1e-6),
         reads=[("ss", tt) for tt in range(NT)], writes=["srt"])
    P.op("dve", lambda e: e.reciprocal(out=rstd, in_=srt), reads=["srt"], writes=["rstd"])
    ycf = YC[:].bitcast(F32)
    obuf = [ycf[:, 0:1024], ycf[:, 1024:2048]]
    okeys = []
    for tt in range(NT):
        ob = tt % 2
        P.op("dve", lambda e, tt=tt, ob=ob: e.scalar_tensor_tensor(out=obuf[ob], in0=Xv[:, tt, :],
                                                                   scalar=rstd[:, tt:tt + 1], in1=fgr, op0=ALU.mult,
                                                                   op1=ALU.mult),
             reads=[("x", tt), "rstd", "fgr"], writes=[("obuf", ob)])
        P.op("sp", dma(out_d[tt * 128:(tt + 1) * 128, :], obuf[ob]), reads=[("obuf", ob)], writes=[("out", tt)], dma=True)
        okeys.append(("out", tt))
    P.op("sp", lambda e: e.nop(), reads=okeys + [k for k in P.state if isinstance(k, str) and k.startswith("dbg")],
         writes=["done"])
    P.emit()
    es.close()
    return nc


def host_consts():
    slopes = np.array([2.0 ** (-8.0 * (h + 1) / 8) for h in range(8)], dtype=np.float64)
    ident = np.eye(128, dtype=np.float32)
    selB = np.zeros((128, 16, 128), np.float32)
    for j in range(16):
        selB[j, j, :] = 1.0
        e = j // 8
        selB[16 + 2 * e, j, :] = 1.0
        selB[17 + 2 * e, j, :] = 1.0
    cm = np.zeros((128, 4, 512), np.float32)
    ik = np.arange(128)[:, None]
    iq = np.arange(512)[None, :]
    for j in range(4):
        cm[:, j, :] = np.where(iq >= 128 * j + ik, 0.0, NEG)
    q = np.arange(T)
    ar = np.zeros((4, 4, T), np.float32)
    for c in range(4):
        for e in range(2):
            h = 2 * c + e
            ar[c, 2 * e, :] = -slopes[h] * 128.0 * (q // 128)
            ar[c, 2 * e + 1, :] = -slopes[h] * (q % 128)
    kb = np.zeros((128, 8, 16), np.float32)
    p = np.arange(128)
    for h in range(8):
        for kt in range(16):
            kb[:, h, kt] = slopes[h] * (128.0 * kt + p)
    iota = np.tile(np.arange(16, dtype=np.float32)[None, :], (128, 1))
    return {"c_ident": ident, "c_selB": selB.reshape(128, 2048), "c_causal": cm.reshape(128, 2048),
            "c_alibi_rows": ar, "c_kb": kb.reshape(128, 128), "c_iota16": iota}


def host_inputs(inputs):
    f = lambda a: np.ascontiguousarray(np.asarray(a, dtype=np.float32))
    shared = {
        "w_ada": f(inputs["w_ada"]),
        "b_ada_rep": f(np.broadcast_to(np.asarray(inputs["b_ada"])[:, None, :], (L, 128, 6 * D))),
        "n1g_rep": f(np.broadcast_to(np.asarray(inputs["norm1_g"])[:, None, :], (L, 128, D))),
        "n2g_rep": f(np.broadcast_to(np.asarray(inputs["norm2_g"])[:, None, :], (L, 128, D))),
        "fg_rep": f(np.broadcast_to(np.asarray(inputs["final_g"])[None, :], (128, D))),
        "w_in": f(inputs["w_in"]),
        "conv_wT": f(np.asarray(inputs["conv_w"]).transpose(0, 2, 1).reshape(L, 4, 128, 3).transpose(0, 2, 1, 3)
                     .reshape(L, 128, 12)),
        "w_attn_proj": f(inputs["w_attn_proj"]),
        "w_conv_proj": f(inputs["w_conv_proj"]),
        "w_out": f(inputs["w_out"]),
        "w_query": f(inputs["w_query"]),
        "keysT": f(np.asarray(inputs["sub_keys"]).transpose(0, 4, 1, 2, 3).reshape(L, 128, 2048)),
    }
    shared["experts_all"] = f(np.concatenate([np.asarray(inputs["expert_u"]).reshape(L * 16384, D),
                                               np.asarray(inputs["expert_v"]).reshape(L * 16384, D)], axis=1))
    shared.update(host_consts())
    x = np.asarray(inputs["x"], dtype=np.float32)
    c = np.asarray(inputs["c"], dtype=np.float32)
    maps = []
    for b in range(x.shape[0]):
        m = dict(shared)
        m["x"] = np.ascontiguousarray(x[b])
        m["crep"] = f(np.broadcast_to(c[b][:, None], (D, 128)))
        maps.append(m)
    return maps


_NC_CACHE = {}


def kernel(**inputs):
    maps = host_inputs(inputs)
    if "nc" not in _NC_CACHE:
        _NC_CACHE["nc"] = build()
    nc = _NC_CACHE["nc"]
    res = run_bass_kernel_spmd(nc, maps, core_ids=list(range(8)))
    out = np.stack([np.asarray(r["out"], dtype=np.float32) for r in res.results], axis=0)
    return out
```

```python
import numpy as np
from contextlib import ExitStack
import concourse.bass as bass
import concourse.mybir as mybir
from concourse.bass_utils import run_bass_kernel_spmd

F32 = mybir.dt.float32
BF16 = mybir.dt.bfloat16
I32 = mybir.dt.int32
U32 = mybir.dt.uint32
AF = mybir.ActivationFunctionType
ALU = mybir.AluOpType
AX = mybir.AxisListType

L = 2
T = 2048
D = 1024
NT = 16
NEG = -30000.0


class Prog:
    ENGS = ["pe", "act", "dve", "pool", "sp"]
    EPOCH = 30000

    def __init__(self, nc, n_dma_sems=32):
        self.nc = nc
        self.streams = {e: [] for e in self.ENGS}
        self.tick = {e: 0 for e in self.ENGS}
        self.ticksems = {e: [] for e in self.ENGS}
        self.dsems = [nc.alloc_semaphore(name=f"dq{i}") for i in range(n_dma_sems)]
        self.dtarget = [0] * n_dma_sems
        self.dnext = 0
        self.hsems = [nc.alloc_semaphore(name=f"hq{i}") for i in range(16)]
        self.htarget = [0] * 16
        self.hnext = 0
        self.state = {}
        self.seen = {e: {} for e in self.ENGS}
        self.semname = {}
        self.bsems = [nc.alloc_semaphore(name=f"bg{i}") for i in range(16)]
        self.btarget = [0] * 16
        self.bnext = 0
        self.bg_keys = set()

    def _ticket(self, eng):
        self.tick[eng] += 1
        ep = (self.tick[eng] - 1) // self.EPOCH
        while len(self.ticksems[eng]) <= ep:
            self.ticksems[eng].append(self.nc.alloc_semaphore(name=f"tk_{eng}_{len(self.ticksems[eng])}"))
        return (("t", eng, ep), self.tick[eng] - ep * self.EPOCH)

    def _sem(self, key):
        if key[0] == "t":
            return self.ticksems[key[1]][key[2]]
        if key[0] == "b":
            return self.bsems[key[1]]
        if key[0] == "h":
            return self.hsems[key[1]]
        return self.dsems[key[1]]

    def op(self, eng, fn, reads=(), writes=(), dma=False, bg=False):
        deps = {}

        def add(tk, own_ok):
            if tk is None:
                return
            key, val = tk
            if key[0] == "t" and key[1] == eng:
                if eng == "pe" or not own_ok:
                    return
            if deps.get(key, 0) < val:
                deps[key] = val

        for k in reads:
            st = self.state.get(k)
            if st:
                add(st["w"], True)
        for k in writes:
            st = self.state.get(k)
            if st:
                add(st["w"], True)
                for r in st["r"]:
                    add(r, True)
        if dma and bg:
            si = self.bnext
            self.bnext = (self.bnext + 1) % len(self.bsems)
            if self.btarget[si] > 0:
                add((("b", si), self.btarget[si]), True)
            self.bg_keys.update(writes)
        elif dma and eng == "sp":
            si = self.hnext
            self.hnext = (self.hnext + 1) % len(self.hsems)
            if self.htarget[si] > 0:
                add((("h", si), self.htarget[si]), True)
        elif dma:
            si = self.dnext
            self.dnext = (self.dnext + 1) % len(self.dsems)
            if self.dtarget[si] > 0:
                add((("d", si), self.dtarget[si]), True)
        waits = []
        for key, val in deps.items():
            if self.seen[eng].get(key, 0) >= val:
                continue
            self.seen[eng][key] = val
            waits.append((key, val))
        if dma and bg:
            self.btarget[si] += 16
            tk = (("b", si), self.btarget[si])
            inc = (("b", si), 16)
        elif dma and eng == "sp":
            self.htarget[si] += 16
            tk = (("h", si), self.htarget[si])
            inc = (("h", si), 16)
        elif dma:
            self.dtarget[si] += 16
            tk = (("d", si), self.dtarget[si])
            inc = (("d", si), 16)
        else:
            tk = self._ticket(eng)
            inc = (tk[0], 1)
        for k in reads:
            self.state.setdefault(k, {"w": None, "r": []})["r"].append(tk)
        for k in writes:
            st = self.state.setdefault(k, {"w": None, "r": []})
            st["w"] = tk
            st["r"] = []
        self.streams[eng].append((waits, fn, inc))
        return tk

    def fence(self):
        tks = []
        for e in self.ENGS:
            if self.tick[e] > 0:
                ep = (self.tick[e] - 1) // self.EPOCH
                tks.append((("t", e, ep), self.tick[e] - ep * self.EPOCH))
        for si, tv in enumerate(self.dtarget):
            if tv > 0:
                tks.append((("d", si), tv))
        for si, tv in enumerate(self.htarget):
            if tv > 0:
                tks.append((("h", si), tv))
        for e in self.ENGS:
            waits = []
            for key, val in tks:
                if key[0] == "t" and key[1] == e:
                    continue
                if self.seen[e].get(key, 0) >= val:
                    continue
                self.seen[e][key] = val
                waits.append((key, val))
            if waits:
                self.streams[e].append((waits, None, None))
        self.state = {k: v for k, v in self.state.items() if k in self.bg_keys}

    def emit(self):
        nc = self.nc
        with nc.Block() as block:
            def body(ename):
                def run(eng):
                    for waits, fn, inc in self.streams[ename]:
                        for key, val in waits:
                            eng.wait_ge(self._sem(key), val)
                        if fn is None:
                            continue
                        ins = fn(eng)
                        ins.then_inc(self._sem(inc[0]), inc[1])
                return run
            block.tensor(body("pe"))
            block.scalar(body("act"))
            block.vector(body("dve"))
            block.gpsimd(body("pool"))
            block.sync(body("sp"))


def mmgroup(items):
    def fn(e):
        ins = None
        for (o, l, r, s, t) in items:
            ins = e.matmul(o, l, r, start=s, stop=t)
        return ins
    return fn


def trgroup(items):
    def fn(e):
        ins = None
        for (o, i, idn) in items:
            ins = e.transpose(o, i, idn)
        return ins
    return fn


def dma(out, in_):
    return lambda e: e.dma_start(out=out, in_=in_)


def build(nlayers=L, dbg=False, dbg_layer=0, late=False):
    nc = bass.Bass("TRN2", target_bir_lowering=False)
    es = ExitStack()

    def din(name, shape, dt=F32):
        return nc.dram_tensor(name, list(shape), dt, kind="ExternalInput").ap()

    x_d = din("x", [T, D])
    crep_d = din("crep", [D, 128])
    w_ada_d = din("w_ada", [L, D, 6 * D])
    b_ada_d = din("b_ada_rep", [L, 128, 6 * D])
    n1g_d = din("n1g_rep", [L, 128, D])
    n2g_d = din("n2g_rep", [L, 128, D])
    fg_d = din("fg_rep", [128, D])
    w_in_d = din("w_in", [L, D, 5120])
    convw_d = din("conv_wT", [L, 128, 12])
    wap_d = din("w_attn_proj", [L, 512, D])
    wcp_d = din("w_conv_proj", [L, 512, D])
    wout_d = din("w_out", [L, D, D])
    wq_d = din("w_query", [L, D, 2048])
    keysT_d = din("keysT", [L, 128, 2048])
    eall_d = din("experts_all", [L * 16384, 2 * D])
    ebf_d = nc.dram_tensor("ebf", [L * 16384, 2 * D], BF16, kind="Internal").ap()
    ident_d = din("c_ident", [128, 128])
    selB_d = din("c_selB", [128, 2048])
    cm_d = din("c_causal", [128, 2048])
    ar_d = din("c_alibi_rows", [4, 4, T])
    kb_d = din("c_kb", [128, 128])
    iota_d = din("c_iota16", [128, 16])
    out_d = nc.dram_tensor("out", [T, D], F32, kind="ExternalOutput").ap()
    dbg_d = {}
    if late:
        dbg_d["d_eidx"] = nc.dram_tensor("d_eidx", [128, 2048], I32, kind="ExternalOutput").ap()
        dbg_d["d_gates"] = nc.dram_tensor("d_gates", [128, 2048], F32, kind="ExternalOutput").ap()
    if dbg:
        for nm, shp, dt in [("d_hT", [128, 16384], BF16), ("d_yaT", [128, 8192], BF16), ("d_ycT", [128, 8192], BF16),
                            ("d_x1", [128, 16384], F32), ("d_eidx", [128, 2048], I32), ("d_gates", [128, 2048], F32),
                            ("d_x2", [128, 16384], F32), ("d_x0", [128, 16384], F32), ("d_ada", [128, 6144], F32), ("d_v", [128, 8192], BF16)]:
            dbg_d[nm] = nc.dram_tensor(nm, shp, dt, kind="ExternalOutput").ap()

    def sb(name, shape, dt):
        return es.enter_context(nc.sbuf_tensor(name, list(shape), dt))

    X = sb("X", [128, NT, D], F32)
    ADA = sb("ADA", [128, 6 * D], F32)
    HT = sb("HT", [128, 16384], BF16)
    R1 = sb("R1", [128, 16384], BF16)
    YA = sb("YA", [128, 8192], BF16)
    YC = sb("YC", [128, 8192], BF16)
    S = sb("S", [128, 4096], BF16)
    W = sb("W", [128, 4096], BF16)
    ident = sb("ident", [128, 128], BF16)
    onesb = sb("onesb", [128, 128], BF16)
    kb = sb("kb", [128, 128], F32)
    iota16 = sb("iota16", [128, 16], F32)
    small = sb("small", [128, 256], F32)
    cw = sb("cw", [128, 12], F32)
    smallR = sb("smallR", [128, 2 * 3 * 96], F32)
    tails = sb("tails", [128, 512], F32)
    iotx = sb("iotx", [128, 16], F32)
    PS = es.enter_context(nc.psum_tensor("PS", [128, 8, 512], F32))

    P = Prog(nc)
    Xv = X[:]
    ADAv = ADA[:]
    hT = HT[:].rearrange("p (c t) -> p c t", c=8)
    PSv = PS[:]

    def psbf(bank):
        return PSv[:, bank, :].bitcast(BF16)

    ss = small[:, 0:16]
    srt = small[:, 16:32]
    rstd = small[:, 32:48]

    P.op("pool", dma(ident[:], ident_d), writes=["ident"], dma=True)
    P.op("sp", dma(kb[:], kb_d), writes=["kb"], dma=True)
    P.op("sp", dma(iota16[:], iota_d), writes=["iota16"], dma=True)
    P.op("pool", lambda e: e.memset(onesb[:], 1.0), writes=["onesb"])
    P.op("dve", lambda e: e.tensor_scalar(out=iotx[:], in0=iota16[:], scalar1=16.0, scalar2=None, op0=ALU.mult),
         reads=["iota16"], writes=["iotx"])
    for tt in range(NT):
        P.op("sp", dma(Xv[:, tt, :], x_d[tt * 128:(tt + 1) * 128, :]), writes=[("x", tt)], dma=True)

    CR = 512
    ncast = (nlayers * 16384) // CR
    cast_i = [0]

    def bg_pump(n=1):
        for _ in range(n):
            ck = cast_i[0]
            if ck >= ncast:
                return
            cast_i[0] += 1
            P.op("pool", dma(ebf_d[ck * CR:(ck + 1) * CR, :], eall_d[ck * CR:(ck + 1) * CR, :]),
                 writes=[("ebf", ck)], dma=True, bg=True)

    pump_n = [0]

    Wt = [W[:, j * 1024:(j + 1) * 1024].rearrange("p (k n) -> p k n", k=8) for j in range(4)]
    Wwhole = W[:].rearrange("p (k n) -> p k n", k=8)
    wkeys_all = [("W4", j) for j in range(4)]
    wrot = [0]

    def next_w():
        j = wrot[0] % 4
        wrot[0] += 1
        return j

    def load_wtile(j, src):
        P.op("pool", dma(Wt[j], src.rearrange("(k p) n -> p k n", p=128)), writes=[("W4", j)], dma=True)
        bg_pump(pump_n[0])

    def norm_phase(A_ap, B_ap, hrow_of, keep_rows):
        ycf = YC[:].bitcast(F32)
        tmpn = [ycf[:, 0:1024], ycf[:, 1024:2048]]
        junk = YC[:, 4096:5120]
        for tt in range(NT):
            P.op("act", lambda e, tt=tt: e.activation(out=junk, in_=Xv[:, tt, :], func=AF.Square,
                                                     accum_out=ss[:, tt:tt + 1]),
                 reads=[("x", tt)], writes=["junk", ("ss", tt)])
        P.op("act", lambda e: e.activation(out=srt, in_=ss, func=AF.Sqrt, scale=1.0 / D, bias=1e-6),
             reads=[("ss", tt) for tt in range(NT)], writes=["srt"])
        P.op("dve", lambda e: e.reciprocal(out=rstd, in_=srt), reads=["srt"], writes=["rstd"])
        for tt in range(NT):
            tb = tt % 2
            hr = hrow_of(tt)
            P.op("dve", lambda e, tt=tt, tb=tb: e.scalar_tensor_tensor(
                out=tmpn[tb], in0=Xv[:, tt, :], scalar=rstd[:, tt:tt + 1], in1=A_ap, op0=ALU.mult, op1=ALU.mult),
                reads=[("x", tt), "rstd", "adaA"], writes=[("tmpn", tb)])
            P.op("pool", lambda e, tb=tb, hr=hr: e.tensor_tensor(out=hr[0], in0=tmpn[tb], in1=B_ap, op=ALU.add),
                 reads=[("tmpn", tb), "adaB"], writes=[hr[1]])
            bank = tt % 2
            pb = psbf(bank)
            P.op("pe", trgroup([(pb[:, c * 128:(c + 1) * 128], hr[0][:, c * 128:(c + 1) * 128], ident[:])
                                for c in range(8)]),
                 reads=[hr[1], "ident"], writes=[("ps", bank)])
            P.op("act", lambda e, tt=tt, pb=pb: e.copy(out=hT[:, :, tt * 128:(tt + 1) * 128],
                                                      in_=pb.rearrange("p (c t) -> p c t", c=8)),
                 reads=[("ps", bank)], writes=[("hT", tt)])

    for l in range(nlayers):
        P.fence()
        if dbg and l == dbg_layer:
            P.op("sp", dma(dbg_d["d_x0"], X[:].rearrange("p t d -> p (t d)")), reads=[], writes=["dbg_x0"], dma=True)
            P.fence()
        wst = HT[:].bitcast(F32).rearrange("p (b k n) -> p b k n", b=2, k=8)
        crep = R1[:, 0:2048].bitcast(F32).rearrange("p (k m) -> p k m", k=8)
        brep = R1[:, 2048:4096].bitcast(F32).rearrange("p (b n) -> p b n", b=2)
        grep = R1[:, 4096:8192].bitcast(F32).rearrange("p (b n) -> p b n", b=2)
        P.op("sp", dma(crep, crep_d.rearrange("(k p) m -> p k m", p=128)), writes=["crep"], dma=True)
        P.op("sp", dma(grep[:, 0, :], n1g_d[l]), writes=[("grep", 0)], dma=True)
        P.op("sp", dma(grep[:, 1, :], n2g_d[l]), writes=[("grep", 1)], dma=True)
        P.op("sp", dma(cw[:], convw_d[l]), writes=["cw"], dma=True)
        wada_v = w_ada_d[l].rearrange("(k p) n -> p k n", p=128)
        for nt in range(12):
            b = nt % 2
            P.op("sp", dma(wst[:, b], wada_v[:, :, nt * 512:(nt + 1) * 512]), writes=[("wst", b)], dma=True)
            P.op("sp", dma(brep[:, b, :], b_ada_d[l][:, nt * 512:(nt + 1) * 512]), writes=[("brep", b)], dma=True)
            P.op("pe", mmgroup([(PSv[:, b, :], crep[:, k, :], wst[:, b, k, :], k == 0, k == 7) for k in range(8)]),
                 reads=[("wst", b), "crep"], writes=[("ps", b)])
            P.op("dve", lambda e, b=b, nt=nt: e.tensor_tensor(out=ADAv[:, nt * 512:(nt + 1) * 512], in0=PSv[:, b, :],
                                                              in1=brep[:, b, :], op=ALU.add),
                 reads=[("ps", b), ("brep", b)], writes=[("ada", nt)])
        P.op("dve", lambda e: e.scalar_tensor_tensor(out=ADAv[:, 1024:2048], in0=ADAv[:, 1024:2048], scalar=1.0,
                                                     in1=grep[:, 0, :], op0=ALU.add, op1=ALU.mult),
             reads=[("ada", 2), ("ada", 3), ("grep", 0)], writes=[("ada", 2), ("ada", 3)])
        P.op("dve", lambda e: e.scalar_tensor_tensor(out=ADAv[:, 4096:5120], in0=ADAv[:, 4096:5120], scalar=1.0,
                                                     in1=grep[:, 1, :], op0=ALU.add, op1=ALU.mult),
             reads=[("ada", 8), ("ada", 9), ("grep", 1)], writes=[("ada", 8), ("ada", 9)])
        if dbg and l == dbg_layer:
            P.op("sp", dma(dbg_d["d_ada"], ADAv), reads=[("ada", i) for i in range(12)], writes=["dbg_ada"], dma=True)
        P.fence()
        SH1, A1, G1 = ADAv[:, 0:1024], ADAv[:, 1024:2048], ADAv[:, 2048:3072]
        SH2, A2, G2 = ADAv[:, 3072:4096], ADAv[:, 4096:5120], ADAv[:, 5120:6144]

        hrows = [YC[:, 5120:6144], YC[:, 6144:7168]]
        norm_phase(A1, SH1, lambda tt: (hrows[tt % 2], ("hrow", tt % 2)), False)
        P.fence()
        if dbg and l == dbg_layer:
            P.op("sp", dma(dbg_d["d_hT"], HT[:]), reads=[("hT", tt) for tt in range(NT)], writes=["dbg_hT"], dma=True)
        win = w_in_d[l]
        v_sb = R1[:, 0:8192].rearrange("p (t n) -> p t n", t=16)
        qslots = R1[:, 8192:16384].rearrange("p (s t) -> p s t", s=4)
        qz = [qslots[:, 1, :], qslots[:, 2, :]]
        P.op("pool", lambda e: e.memset(qz[0][64:128, :], 0.0), writes=[("qzero", 0)])
        P.op("pool", lambda e: e.memset(qz[1][0:64, :], 0.0), writes=[("qzero", 1)])
        P.op("pool", dma(Wwhole, win[:, 1024:1536].rearrange("(k p) n -> p k n", p=128)), writes=wkeys_all, dma=True)
        for tt in range(NT):
            bank = tt % 2
            P.op("pe", mmgroup([(PSv[:, bank, :], hT[:, k, tt * 128:(tt + 1) * 128], Wwhole[:, k, :], k == 0, k == 7)
                                for k in range(8)]),
                 reads=[("hT", tt)] + wkeys_all, writes=[("ps", bank)])
            P.op("act", lambda e, tt=tt, bank=bank: e.copy(out=v_sb[:, tt, :], in_=PSv[:, bank, :]),
                 reads=[("ps", bank)], writes=[("v", tt)])
        if dbg and l == dbg_layer:
            P.op("sp", dma(dbg_d["d_v"], R1[:, 0:8192]), reads=[("v", tt) for tt in range(NT)], writes=["dbg_v"], dma=True)
        Rc = [YC[:, 0:2048], YC[:, 2048:4096]]
        selB = YC[:, 4096:6144].rearrange("p (j m) -> p j m", j=16)
        CM = YC[:, 6144:8192].rearrange("p (j q) -> p j q", j=4)
        P.op("pool", dma(YC[:, 4096:6144], selB_d), writes=["selB"], dma=True)
        P.op("pool", dma(YC[:, 6144:8192], cm_d), writes=["CM"], dma=True)
        PT = [S[:, i * 512:(i + 1) * 512] for i in range(3)]
        rcp = S[:, 2048:3072].bitcast(F32)
        gsb = small[:, 64:80]
        m8 = small[:, 80:96]
        mb32 = small[:, 96:112]
        km32 = small[:, 112:120]
        mbb = S[:, 3072:3088]
        kmT = S[:, 3104:3112]
        yaT = YA[:].rearrange("p (c t) -> p c t", c=4)
        sbi = [0]
        pti = [0]
        pump_n[0] = 0
        for c in range(4):
            cb = c % 2
            kT = qslots[:, 0, :]
            jq = next_w()
            load_wtile(jq, win[:, c * 128:(c + 1) * 128])
            jk = next_w()
            load_wtile(jk, win[:, 512 + c * 128:512 + (c + 1) * 128])
            if l == 0:
                bg_pump(8)
            P.op("pool", lambda e, cb=cb: e.memset(Rc[cb], 0.0),
                 writes=[("Rc", cb, tt) for tt in range(NT)] + [("RcA", cb)])
            P.op("pool", dma(Rc[cb][16:20, :], ar_d[c]), writes=[("RcA", cb)], dma=True)
            for Q in range(4):
                P.op("pe", mmgroup([(PSv[:, 6, :], Wt[jk][:, k, :], hT[:, k, Q * 512:(Q + 1) * 512], k == 0, k == 7)
                                    for k in range(8)]),
                     reads=[("W4", jk)] + [("hT", 4 * Q + i) for i in range(4)], writes=[("ps", 6)])
                P.op("act", lambda e, Q=Q, kT=kT: e.copy(out=kT[:, Q * 512:(Q + 1) * 512], in_=PSv[:, 6, :]),
                     reads=[("ps", 6)], writes=[("kT", 0, Q)])
                P.op("pe", mmgroup([(PSv[:, 7, :], Wt[jq][:, k, :], hT[:, k, Q * 512:(Q + 1) * 512], k == 0, k == 7)
                                    for k in range(8)]),
                     reads=[("W4", jq)] + [("hT", 4 * Q + i) for i in range(4)], writes=[("ps", 7)])
                for e8 in range(2):
                    P.op("act", lambda e, Q=Q, e8=e8: e.mul(out=qz[e8][64 * e8:64 * e8 + 64, Q * 512:(Q + 1) * 512],
                                                            in_=PSv[64 * e8:64 * e8 + 64, 7, :], mul=0.125),
                         reads=[("ps", 7)], writes=[("qT", 0, Q, e8)])
            P.op("dve", lambda e, kT=kT: e.tensor_reduce(out=km32, in_=kT.rearrange("p (n s) -> p n s", n=8),
                                                         axis=AX.X, op=ALU.add),
                 reads=[("kT", 0, Q) for Q in range(4)], writes=["km32"])
            P.op("dve", lambda e: e.tensor_scalar(out=kmT, in0=km32, scalar1=1.0 / 256, scalar2=None, op0=ALU.mult),
                 reads=["km32"], writes=["kmT"])
            for tt in range(8, NT):
                qb = tt // 2
                P.op("pe", mmgroup([(PSv[:, 5, e8 * 8:(e8 + 1) * 8],
                                     qz[e8][64 * e8:64 * e8 + 64, tt * 128:(tt + 1) * 128],
                                     kmT[64 * e8:64 * e8 + 64, :], True, True) for e8 in range(2)]),
                     reads=[("qT", 0, tt // 4, 0), ("qT", 0, tt // 4, 1), "kmT"], writes=[("ps", 5)])
                P.op("dve", lambda e: e.tensor_copy(out=gsb, in_=PSv[:, 5, 0:16]), reads=[("ps", 5)], writes=["gsb"])
                P.op("dve", lambda e, qb=qb: e.memset(gsb.rearrange("p (a n) -> p a n", a=2)[:, :, qb:8], -1e30),
                     reads=["gsb"], writes=["gsb"])
                for e8 in range(2):
                    P.op("dve", lambda e, e8=e8: e.max(out=m8[:, e8 * 8:(e8 + 1) * 8], in_=gsb[:, e8 * 8:(e8 + 1) * 8]),
                         reads=["gsb"], writes=[("m8", e8)])
                for e8 in range(2):
                    P.op("dve", lambda e, e8=e8: e.tensor_scalar(out=mb32[:, e8 * 8:(e8 + 1) * 8],
                                                                 in0=gsb[:, e8 * 8:(e8 + 1) * 8],
                                                                 scalar1=m8[:, e8 * 8 + 2:e8 * 8 + 3], scalar2=None,
                                                                 op0=ALU.is_ge),
                         reads=["gsb", ("m8", e8)], writes=[("mb32", e8)])
                P.op("dve", lambda e: e.tensor_scalar(out=mbb, in0=mb32, scalar1=-1.0, scalar2=-NEG, op0=ALU.add,
                                                      op1=ALU.mult),
                     reads=[("mb32", 0), ("mb32", 1)], writes=["mbb"])
                P.op("dve", lambda e, qb=qb: e.memset(mbb.rearrange("p (a n) -> p a n", a=2)[:, :, qb:8], 0.0),
                     reads=["mbb"], writes=["mbb"])
                pb5 = psbf(5)
                P.op("pe", trgroup([(pb5[0:16, 512:640], mbb, ident[:])]), reads=["mbb", "ident"], writes=[("ps", 5)])
                P.op("act", lambda e, tt=tt, cb=cb, pb5=pb5: e.copy(out=Rc[cb][0:16, tt * 128:(tt + 1) * 128],
                                                                   in_=pb5[0:16, 512:640]),
                     reads=[("ps", 5)], writes=[("Rc", cb, tt)])
            steps = [(e8, Q, kt) for Q in range(4) for e8 in range(2) for kt in range(4 * Q + 4)]
            pend = None

            def emit_pv(st):
                e8, Q, kt, ptb = st
                nkt = 4 * Q + 4
                lo, hi = 64 * e8, 64 * e8 + 64
                P.op("pe", mmgroup([(PSv[:, 3, :], v_sb[:, kt, c * 128:(c + 1) * 128], PT[ptb], kt == 0,
                                     kt == nkt - 1),
                                    (PSv[:, 4, :], onesb[:], PT[ptb], kt == 0, kt == nkt - 1)]),
                     reads=[("PT", ptb), ("v", kt), "onesb"], writes=[("ps", 3), ("ps", 4)])
                if kt == nkt - 1:
                    P.op("dve", lambda e, lo=lo, hi=hi: e.reciprocal(out=rcp[lo:hi, :], in_=PSv[lo:hi, 4, :]),
                         reads=[("ps", 4)], writes=["rcp"])
                    P.op("dve", lambda e, lo=lo, hi=hi, Q=Q, c=c: e.tensor_tensor(
                        out=yaT[lo:hi, c, Q * 512:(Q + 1) * 512], in0=PSv[lo:hi, 3, :], in1=rcp[lo:hi, :], op=ALU.mult),
                        reads=[("ps", 3), "rcp"], writes=[("yaT", c, Q, e8)])

            for (e8, Q, kt) in steps:
                h = 2 * c + e8
                lo, hi = 64 * e8, 64 * e8 + 64
                n = kt // 2
                sbk = sbi[0] % 3
                sbi[0] += 1
                ptb = pti[0] % 3
                pti[0] += 1
                items = [(PSv[:, sbk, :], kT[:, kt * 128:(kt + 1) * 128], qz[e8][:, Q * 512:(Q + 1) * 512],
                          True, False),
                         (PSv[:, sbk, :], selB[:, e8 * 8 + n, :], Rc[cb][:, Q * 512:(Q + 1) * 512], False,
                          kt < 4 * Q)]
                if kt >= 4 * Q:
                    items.append((PSv[:, sbk, :], ident[:], CM[:, kt - 4 * Q, :], False, True))
                P.op("pe", mmgroup(items),
                     reads=[("kT", 0, kt // 4), ("qT", 0, Q, e8), ("qzero", e8), "selB", "CM", "ident", ("RcA", cb)] +
                           [("Rc", cb, 4 * Q + i) for i in range(4)],
                     writes=[("ps", sbk)])
                P.op("act", lambda e, sbk=sbk, ptb=ptb, h=h, kt=kt: e.activation(
                    out=PT[ptb], in_=PSv[:, sbk, :], func=AF.Exp, bias=kb[:, h * 16 + kt:h * 16 + kt + 1],
                    scale=1.0),
                    reads=[("ps", sbk), "kb"], writes=[("PT", ptb)])
                if pend is not None:
                    emit_pv(pend)
                pend = (e8, Q, kt, ptb)
            emit_pv(pend)
        P.fence()
        if dbg and l == dbg_layer:
            P.op("sp", dma(dbg_d["d_yaT"], YA[:]), reads=[], writes=["dbg_ya"], dma=True)
            P.fence()

        pump_n[0] = 0
        r1f = R1[:].bitcast(F32)
        zf = r1f[:, 0:2050]
        yf = r1f[:, 2304:4352]
        hcs = [r1f[:, 4608:5120], r1f[:, 5120:5632]]
        ycT = YC[:].rearrange("p (c t) -> p c t", c=4)
        P.op("dve", lambda e: e.memset(zf[:, 0:2], 0.0), writes=["z0"])
        for cc in range(4):
            jc = next_w()
            load_wtile(jc, win[:, 2048 + cc * 128:2048 + (cc + 1) * 128])
            jh = next_w()
            load_wtile(jh, win[:, 2560 + cc * 128:2560 + (cc + 1) * 128])
            jb = next_w()
            load_wtile(jb, win[:, 1536 + cc * 128:1536 + (cc + 1) * 128])
            for Q in range(4):
                hb = Q % 2
                bc_, bh_ = 4 * hb, 4 * hb + 1
                P.op("pe", mmgroup([(PSv[:, bc_, :], Wt[jc][:, k, :], hT[:, k, Q * 512:(Q + 1) * 512], k == 0, k == 7)
                                    for k in range(8)]), reads=[("W4", jc)], writes=[("ps", bc_)])
                P.op("pe", mmgroup([(PSv[:, bh_, :], Wt[jh][:, k, :], hT[:, k, Q * 512:(Q + 1) * 512], k == 0, k == 7)
                                    for k in range(8)]), reads=[("W4", jh)], writes=[("ps", bh_)])
                P.op("act", lambda e, hb=hb, bh_=bh_: e.copy(out=hcs[hb], in_=PSv[:, bh_, :]), reads=[("ps", bh_)],
                     writes=[("hcs", hb)])
                P.op("dve", lambda e, hb=hb, Q=Q, bc_=bc_: e.tensor_tensor(out=zf[:, 2 + Q * 512:2 + (Q + 1) * 512],
                                                                          in0=PSv[:, bc_, :], in1=hcs[hb], op=ALU.mult),
                     reads=[("ps", bc_), ("hcs", hb)], writes=[("z", Q)])
            zr = [("z", Q) for Q in range(4)] + ["z0"]
            P.op("dve", lambda e, cc=cc: e.tensor_scalar(out=yf, in0=zf[:, 2:2050], scalar1=cw[:, cc * 3 + 2:cc * 3 + 3],
                                                         scalar2=None, op0=ALU.mult), reads=zr + ["cw"], writes=["y"])
            P.op("dve", lambda e, cc=cc: e.scalar_tensor_tensor(out=yf, in0=zf[:, 1:2049],
                                                                scalar=cw[:, cc * 3 + 1:cc * 3 + 2], in1=yf,
                                                                op0=ALU.mult, op1=ALU.add), reads=zr + ["y"], writes=["y"])
            P.op("dve", lambda e, cc=cc: e.scalar_tensor_tensor(out=yf, in0=zf[:, 0:2048],
                                                                scalar=cw[:, cc * 3:cc * 3 + 1], in1=yf,
                                                                op0=ALU.mult, op1=ALU.add), reads=zr + ["y"], writes=["y"])
            for Q in range(4):
                bank = 2 + Q % 2
                P.op("pe", mmgroup([(PSv[:, bank, :], Wt[jb][:, k, :], hT[:, k, Q * 512:(Q + 1) * 512], k == 0, k == 7)
                                    for k in range(8)]), reads=[("W4", jb)], writes=[("ps", bank)])
                P.op("dve", lambda e, bank=bank, cc=cc, Q=Q: e.tensor_tensor(
                    out=ycT[:, cc, Q * 512:(Q + 1) * 512], in0=PSv[:, bank, :], in1=yf[:, Q * 512:(Q + 1) * 512],
                    op=ALU.mult), reads=[("ps", bank), "y"], writes=[("ycT", cc, Q)])
        P.fence()
        if dbg and l == dbg_layer:
            P.op("sp", dma(dbg_d["d_ycT"], YC[:]), reads=[], writes=["dbg_yc"], dma=True)
            P.fence()

        mT = R1[:].rearrange("p (c t) -> p c t", c=8)
        sf = S[:].bitcast(F32)
        sa = sf[:, 0:512]
        sc = sf[:, 512:1024]
        WAC = [S[:, 2048 + i * 512:2048 + (i + 1) * 512].rearrange("p (k n) -> p k n", k=4) for i in range(4)]
        wap_v = wap_d[l].rearrange("(k p) n -> p k n", p=128)
        wcp_v = wcp_d[l].rearrange("(k p) n -> p k n", p=128)
        it = 0
        for dc in range(8):
            jga = next_w()
            load_wtile(jga, win[:, 3072 + dc * 128:3072 + (dc + 1) * 128])
            jgc = next_w()
            load_wtile(jgc, win[:, 4096 + dc * 128:4096 + (dc + 1) * 128])
            wa = WAC[(dc % 2) * 2]
            wc = WAC[(dc % 2) * 2 + 1]
            P.op("pool", dma(wa, wap_v[:, :, dc * 128:(dc + 1) * 128]), writes=[("WAC", (dc % 2) * 2)], dma=True)
            P.op("pool", dma(wc, wcp_v[:, :, dc * 128:(dc + 1) * 128]), writes=[("WAC", (dc % 2) * 2 + 1)], dma=True)
            for Q in range(4):
                b0 = 4 * (it % 2)
                it += 1
                qs = slice(Q * 512, (Q + 1) * 512)
                P.op("pe", mmgroup([(PSv[:, b0, :], wa[:, k, :], yaT[:, k, qs], k == 0, k == 3) for k in range(4)]),
                     reads=[("WAC", (dc % 2) * 2)], writes=[("ps", b0)])
                P.op("pe", mmgroup([(PSv[:, b0 + 1, :], Wt[jga][:, k, :], hT[:, k, qs], k == 0, k == 7)
                                    for k in range(8)]), reads=[("W4", jga)], writes=[("ps", b0 + 1)])
                P.op("pe", mmgroup([(PSv[:, b0 + 2, :], wc[:, k, :], ycT[:, k, qs], k == 0, k == 3) for k in range(4)]),
                     reads=[("WAC", (dc % 2) * 2 + 1)], writes=[("ps", b0 + 2)])
                P.op("pe", mmgroup([(PSv[:, b0 + 3, :], Wt[jgc][:, k, :], hT[:, k, qs], k == 0, k == 7)
                                    for k in range(8)]), reads=[("W4", jgc)], writes=[("ps", b0 + 3)])
                P.op("act", lambda e, b0=b0: e.activation(out=sa, in_=PSv[:, b0 + 1, :], func=AF.Sigmoid),
                     reads=[("ps", b0 + 1)], writes=["sa"])
                P.op("act", lambda e, b0=b0: e.activation(out=sc, in_=PSv[:, b0 + 3, :], func=AF.Sigmoid),
                     reads=[("ps", b0 + 3)], writes=["sc"])
                P.op("dve", lambda e, b0=b0: e.tensor_tensor(out=sa, in0=sa, in1=PSv[:, b0, :], op=ALU.mult),
                     reads=["sa", ("ps", b0)], writes=["sa"])
                P.op("dve", lambda e, b0=b0: e.tensor_tensor(out=sc, in0=sc, in1=PSv[:, b0 + 2, :], op=ALU.mult),
                     reads=["sc", ("ps", b0 + 2)], writes=["sc"])
                P.op("pool", lambda e, dc=dc, qs=qs: e.tensor_tensor(out=mT[:, dc, qs], in0=sa, in1=sc, op=ALU.add),
                     reads=["sa", "sc"], writes=[("mT", dc, Q)])
        P.fence()
        otmp = [sf[:, 0:512], sf[:, 512:1024]]
        wout_v = wout_d[l].rearrange("(k p) n -> p k n", p=128)
        for half in range(2):
            hs = slice(half * 512, (half + 1) * 512)
            P.op("pool", dma(Wwhole, wout_v[:, :, hs]), writes=wkeys_all, dma=True)
            for tt in range(NT):
                bank = tt % 2
                P.op("pe", mmgroup([(PSv[:, bank, :], mT[:, k, tt * 128:(tt + 1) * 128], Wwhole[:, k, :], k == 0, k == 7)
                                    for k in range(8)]), reads=wkeys_all, writes=[("ps", bank)])
                P.op("dve", lambda e, bank=bank, hs=hs: e.tensor_tensor(out=otmp[bank], in0=PSv[:, bank, :],
                                                                        in1=G1[:, hs], op=ALU.mult),
                     reads=[("ps", bank)], writes=[("otmp", bank)])
                P.op("pool", lambda e, bank=bank, hs=hs, tt=tt: e.tensor_tensor(out=Xv[:, tt, hs], in0=Xv[:, tt, hs],
                                                                                in1=otmp[bank], op=ALU.add),
                     reads=[("otmp", bank), ("x", tt)], writes=[("x", tt)])
        P.fence()
        if dbg and l == dbg_layer:
            P.op("sp", dma(dbg_d["d_x1"], X[:].rearrange("p t d -> p (t d)")), reads=[], writes=["dbg_x1"], dma=True)
            P.fence()

        H2 = R1[:].rearrange("p (t d) -> p t d", t=16)
        norm_phase(A2, SH2, lambda tt: (H2[:, tt, :], ("h2", tt)), True)
        P.fence()
        keysT = S[:, 0:2048].rearrange("p (j n) -> p j n", j=16)
        P.op("pool", dma(S[:, 0:2048], keysT_d[l]), writes=["keysT"], dma=True)
        q2T = [YC[:, 0:4096].rearrange("p (w t) -> p w t", w=2), YC[:, 4096:8192].rearrange("p (w t) -> p w t", w=2)]
        eidx = YA[:, 0:4096].bitcast(I32).rearrange("p (t k) -> p t k", t=16)
        gates = YA[:, 4096:8192].bitcast(F32).rearrange("p (t k) -> p t k", t=16)
        wqv = wq_d[l]

        smR = smallR[:].rearrange("p (g c w) -> p g c w", g=2, c=3)

        def mk_scratch(cid, par):
            base = [S[:, 2048:4096].bitcast(F32), W[:, 2048:4096].bitcast(F32), ADAv[:, 3072:4096]][cid]
            sm = smR[:, par, cid, :]
            return dict(scs=base[:, 0:256], scw=base[:, 256:512], cand=base[:, 512:768], candw=base[:, 768:1024],
                        vv=sm[:, 0:32], ixu=sm[:, 32:64].bitcast(U32), bv=sm[:, 64:80],
                        posu=sm[:, 80:96].bitcast(U32), cid=cid, par=par)

        def chain(h, tt, sc, qb2):
            KS_ = lambda name, *a: (name, sc["cid"]) + tuple(a)
            K_ = lambda name, *a: (name, sc["cid"], sc["par"]) + tuple(a)
            scs, scw, cand, candw = sc["scs"], sc["scw"], sc["cand"], sc["candw"]
            vv, ixu, bv, posu = sc["vv"], sc["ixu"], sc["bv"], sc["posu"]
            bank = 2 + tt % 2
            ts_ = slice(tt * 128, (tt + 1) * 128)
            P.op("pe", mmgroup([(PSv[:, bank, p2 * 128:(p2 + 1) * 128], qb2[:, p2, ts_], keysT[:, 2 * h + p2, :],
                                 True, True) for p2 in range(2)]),
                 reads=[("q2T", h % 2, p2, tt // 4) for p2 in range(2)] + ["keysT"], writes=[("ps", bank)])
            P.op("act", lambda e: e.copy(out=scs, in_=PSv[:, bank, 0:256]), reads=[("ps", bank)], writes=[KS_("scs")])
            yield
            for p2 in range(2):
                sl = slice(p2 * 128, (p2 + 1) * 128)
                v0 = slice(p2 * 16, p2 * 16 + 8)
                v1 = slice(p2 * 16 + 8, p2 * 16 + 16)
                P.op("dve", lambda e, v0=v0, sl=sl: e.max(out=vv[:, v0], in_=scs[:, sl]), reads=[KS_("scs")], writes=[K_("vv", p2, 0)])
                yield
                P.op("dve", lambda e, v0=v0, sl=sl: e.max_index(out=ixu[:, v0], in_max=vv[:, v0], in_values=scs[:, sl]),
                     reads=[KS_("scs"), K_("vv", p2, 0)], writes=[K_("ix", p2, 0)])
                yield
                P.op("dve", lambda e, v0=v0, sl=sl: e.match_replace(out=scw[:, sl], in_to_replace=vv[:, v0], in_values=scs[:, sl],
                                                      imm_value=-1e30),
                     reads=[KS_("scs"), K_("vv", p2, 0)], writes=[KS_("scw", p2)])
                yield
                P.op("dve", lambda e, v1=v1, sl=sl: e.max(out=vv[:, v1], in_=scw[:, sl]), reads=[KS_("scw", p2)],
                     writes=[K_("vv", p2, 1)])
                yield
                P.op("dve", lambda e, v1=v1, sl=sl: e.max_index(out=ixu[:, v1], in_max=vv[:, v1], in_values=scw[:, sl]),
                     reads=[KS_("scw", p2), K_("vv", p2, 1)], writes=[K_("ix", p2, 1)])
                yield
            vkeys = [K_("vv", a, b) for a in range(2) for b in range(2)]
            ikeys = [K_("ix", a, b) for a in range(2) for b in range(2)]
            c3 = cand.rearrange("p (a b) -> p a b", a=16)
            P.op("dve", lambda e: e.tensor_tensor(out=c3, in0=vv[:, 0:16].unsqueeze(2).to_broadcast([128, 16, 16]),
                                                  in1=vv[:, 16:32].unsqueeze(1).to_broadcast([128, 16, 16]),
                                                  op=ALU.add), reads=vkeys, writes=[KS_("cand")])
            yield
            P.op("dve", lambda e: e.max(out=bv[:, 0:8], in_=cand), reads=[KS_("cand")], writes=[K_("bv", 0)])
            yield
            P.op("dve", lambda e: e.max_index(out=posu[:, 0:8], in_max=bv[:, 0:8], in_values=cand),
                 reads=[KS_("cand"), K_("bv", 0)], writes=[K_("pos", 0)])
            yield
            P.op("dve", lambda e: e.match_replace(out=candw, in_to_replace=bv[:, 0:8], in_values=cand,
                                                  imm_value=-1e30), reads=[KS_("cand"), K_("bv", 0)],
                 writes=[KS_("candw")])
            yield
            P.op("dve", lambda e: e.max(out=bv[:, 8:16], in_=candw), reads=[KS_("candw")], writes=[K_("bv", 1)])
            yield
            P.op("dve", lambda e: e.max_index(out=posu[:, 8:16], in_max=bv[:, 8:16], in_values=candw),
                 reads=[KS_("candw"), K_("bv", 1)], writes=[K_("pos", 1)])
            yield

        eqB = ADAv[:, 4096:4864]

        def tail(h, t0, n, par):
            KB = lambda name, *a: ("tl", name) + tuple(a)
            ck = lambda name, *a: [(name, c_, par) + tuple(a) for c_ in range(n)]
            sm = smR[:, par, 0:n, :]
            ixuB = sm[:, :, 32:64].bitcast(U32)
            bvB = sm[:, :, 64:80]
            posuB = sm[:, :, 80:96].bitcast(U32)
            m = n * 16
            posf = tails[:, 0:m]
            a16 = tails[:, 48:48 + m]
            bq = tails[:, 96:96 + m]
            i1 = tails[:, 144:144 + m]
            i2 = tails[:, 192:192 + m]
            ef = tails[:, 240:240 + m]
            ixf = tails[:, 288:288 + 2 * m].rearrange("p (c w) -> p c w", c=n)
            ex = tails[:, 384:384 + m].rearrange("p (c w) -> p c w", c=n)
            nm = tails[:, 432:432 + n]
            ssum = tails[:, 440:440 + n]
            rs = tails[:, 448:448 + n]
            eq2 = eqB[:, 0:m * 16].rearrange("p (k a) -> p k a", a=16)
            eq3 = eqB[:, 0:m * 16].rearrange("p (c k a) -> p c k a", c=n, a=16)
            v3 = lambda ap: ap.rearrange("p (c w) -> p c w", c=n)
            bc_k = lambda ap: ap.unsqueeze(2).to_broadcast([128, m, 16])
            bc_a = lambda ap: ap.unsqueeze(1).to_broadcast([128, m, 16])
            P.op("dve", lambda e: e.tensor_copy(out=v3(posf), in_=posuB), reads=ck("pos", 0) + ck("pos", 1),
                 writes=[KB("posf")])
            yield
            P.op("dve", lambda e: e.scalar_tensor_tensor(out=eq2, in0=bc_a(iota16[:]), scalar=16.0, in1=bc_k(posf),
                                                         op0=ALU.mult, op1=ALU.is_le),
                 reads=[KB("posf")], writes=[KB("eq")])
            yield
            P.op("dve", lambda e: e.tensor_reduce(out=a16, in_=eq2, axis=AX.X, op=ALU.add), reads=[KB("eq")],
                 writes=[KB("a16")])
            yield
            P.op("act", lambda e: e.activation(out=a16, in_=a16, func=AF.Identity, scale=16.0, bias=-16.0),
                 reads=[KB("a16")], writes=[KB("a16")])
            yield
            P.op("dve", lambda e: e.tensor_tensor(out=bq, in0=posf, in1=a16, op=ALU.subtract),
                 reads=[KB("posf"), KB("a16")], writes=[KB("bq")])
            yield
            P.op("dve", lambda e: e.tensor_copy(out=ixf, in_=ixuB),
                 reads=[k_ for a_ in range(2) for b_ in range(2) for k_ in ck("ix", a_, b_)], writes=[KB("ixf")])
            yield
            P.op("dve", lambda e, loff=float(l * 16384): e.tensor_scalar(
                out=ixf[:, :, 0:16], in0=ixf[:, :, 0:16], scalar1=128.0, scalar2=loff, op0=ALU.mult, op1=ALU.add),
                 reads=[KB("ixf")], writes=[KB("ixf")])
            yield
            for which in range(2):
                src = a16 if which == 0 else bq
                mul = 16.0 if which == 0 else 1.0
                dst = i1 if which == 0 else i2
                P.op("dve", lambda e, src=src, mul=mul: e.scalar_tensor_tensor(
                    out=eq2, in0=bc_a(iota16[:]), scalar=mul, in1=bc_k(src), op0=ALU.mult, op1=ALU.is_equal),
                    reads=[KB("a16"), KB("bq"), KB("eq")], writes=[KB("eq")])
                yield
                P.op("dve", lambda e, which=which: e.tensor_tensor(
                    out=eq3, in0=eq3,
                    in1=ixf[:, :, which * 16:(which + 1) * 16].unsqueeze(2).to_broadcast([128, n, 16, 16]),
                    op=ALU.mult), reads=[KB("eq"), KB("ixf")], writes=[KB("eq")])
                yield
                P.op("dve", lambda e, dst=dst: e.tensor_reduce(out=dst, in_=eq2, axis=AX.X, op=ALU.add),
                     reads=[KB("eq")], writes=[KB("i12", which)])
                yield
            P.op("dve", lambda e: e.tensor_tensor(out=ef, in0=i1, in1=i2, op=ALU.add),
                 reads=[KB("i12", 0), KB("i12", 1)], writes=[KB("ef")])
            yield
            P.op("dve", lambda e: e.tensor_copy(out=eidx[:, t0:t0 + n, h * 16:(h + 1) * 16], in_=v3(ef)),
                 reads=[KB("ef")], writes=[("eidx", t0 + c_, h) for c_ in range(n)])
            yield
            P.op("act", lambda e: e.mul(out=nm, in_=bvB[:, :, 0], mul=-1.0),
                 reads=ck("bv", 0), writes=[KB("nm")])
            yield
            for c_ in range(n):
                P.op("act", lambda e, c_=c_: e.activation(out=ex[:, c_, :], in_=bvB[:, c_, :], func=AF.Exp,
                                                          bias=nm[:, c_:c_ + 1], scale=1.0,
                                                          accum_out=ssum[:, c_:c_ + 1]),
                     reads=[("bv", c_, par, 0), ("bv", c_, par, 1), KB("nm")], writes=[KB("ex", c_), KB("ssum", c_)])
            yield
            P.op("dve", lambda e: e.reciprocal(out=rs, in_=ssum), reads=[KB("ssum", c_) for c_ in range(n)],
                 writes=[KB("rs")])
            yield
            P.op("dve", lambda e: e.tensor_tensor(out=gates[:, t0:t0 + n, h * 16:(h + 1) * 16], in0=ex,
                                                  in1=rs.unsqueeze(2).to_broadcast([128, n, 16]), op=ALU.mult),
                 reads=[KB("ex", c_) for c_ in range(n)] + [KB("rs")],
                 writes=[("gates", t0 + c_, h) for c_ in range(n)])
            yield

        NCH = 3
        scr = [[mk_scratch(i, par) for i in range(NCH)] for par in range(2)]
        grp = [0]
        pending_tail = [None]

        def run_interleaved(gens):
            alive = [True] * len(gens)
            while any(alive):
                for gi_, g_ in enumerate(gens):
                    if alive[gi_]:
                        try:
                            next(g_)
                        except StopIteration:
                            alive[gi_] = False

        wr2 = 0
        pump_n[0] = 2
        for h in range(8):
            qb2 = q2T[h % 2]
            for p2 in range(2):
                jw = wr2 % 2
                wr2 += 1
                load_wtile(jw, wqv[:, (2 * h + p2) * 128:(2 * h + p2 + 1) * 128])
                for Q in range(4):
                    bank = (2 * p2 + Q) % 2
                    P.op("pe", mmgroup([(PSv[:, bank, :], Wt[jw][:, k, :], hT[:, k, Q * 512:(Q + 1) * 512], k == 0, k == 7)
                                        for k in range(8)]), reads=[("W4", jw)], writes=[("ps", bank)])
                    P.op("act", lambda e, qb2=qb2, p2=p2, Q=Q, bank=bank: e.copy(
                        out=qb2[:, p2, Q * 512:(Q + 1) * 512], in_=PSv[:, bank, :]),
                        reads=[("ps", bank)], writes=[("q2T", h % 2, p2, Q)])
            for t0 in range(0, NT, NCH):
                par = grp[0] % 2
                grp[0] += 1
                n = min(NCH, NT - t0)
                gens = [chain(h, t0 + i, scr[par][i], qb2) for i in range(n)]
                if pending_tail[0] is not None:
                    gens.append(pending_tail[0])
                run_interleaved(gens)
                pending_tail[0] = tail(h, t0, n, par)
        run_interleaved([pending_tail[0]])
        pending_tail[0] = None
        P.fence()
        if dbg and l == dbg_layer:
            P.op("sp", dma(dbg_d["d_eidx"], YA[:, 0:4096].bitcast(I32)), reads=[], writes=["dbg_e"], dma=True)
            P.op("sp", dma(dbg_d["d_gates"], YA[:, 4096:8192].bitcast(F32)), reads=[], writes=["dbg_g"], dma=True)
            P.fence()
        pump_n[0] = 0
        bg_pump(ncast)
        ebf_keys = [("ebf", ck) for ck in range(((l + 1) * 16384) // CR)]
        first_gather = [True]
        GS = 4
        adabf = ADA[:, 0:5120].bitcast(BF16)
        UV = ([HT[:, i * 2048:(i + 1) * 2048] for i in range(8)] +
              [adabf[:, i * 2048:(i + 1) * 2048] for i in range(5)] +
              [W[:, i * 2048:(i + 1) * 2048] for i in range(2)] +
              [S[:, 2048:4096], YC[:, 6144:8192]])
        NB = len(UV)
        junk2 = YC[:, 0:1024]
        ycf = YC[:].bitcast(F32)
        aall = ycf[:, 512:640]
        t1 = ycf[:, 640:768]
        sg = ycf[:, 768:896]
        wgt = ycf[:, 896:1024]
        ga = ycf[:, 1024:1152]
        ptmp = [ycf[:, 1152:1664], ycf[:, 1664:2176]]
        dg = [S[:, i * 128:(i + 1) * 128] for i in range(8)]
        gi = 0
        ngrp = 128 // GS
        for tt in range(NT):
            pb0 = 2 * (tt % 2)
            bufs_of = {}

            def stage1(gq, tt=tt):
                nonlocal gi
                cs = slice(gq * GS, (gq + 1) * GS)
                bl = []
                for hk in range(gq * GS, (gq + 1) * GS):
                    g = gi % NB
                    gi += 1
                    bl.append(g)
                    P.op("pool", lambda e, g=g, hk=hk: e.indirect_dma_start(
                        out=UV[g], out_offset=None, in_=ebf_d,
                        in_offset=bass.IndirectOffsetOnAxis(ap=eidx[:, tt, hk:hk + 1], axis=0)),
                        reads=[("eidx", tt, hk // 16)] + (ebf_keys if first_gather[0] else []),
                        writes=[("UV", g)], dma=True)
                    first_gather[0] = False
                    P.op("dve", lambda e, g=g, hk=hk: e.scalar_tensor_tensor(
                        out=junk2, in0=UV[g][:, 0:1024], scalar=1.0, in1=H2[:, tt, :], op0=ALU.mult, op1=ALU.mult,
                        accum_out=aall[:, hk:hk + 1]),
                        reads=[("UV", g), ("h2", tt)], writes=["junk2", ("a", hk)])
                bufs_of[gq] = bl
                ak = [("a", hk) for hk in range(gq * GS, (gq + 1) * GS)]
                P.op("dve", lambda e: e.scalar_tensor_tensor(out=t1[:, cs], in0=aall[:, cs], scalar=0.044715,
                                                             in1=aall[:, cs], op0=ALU.mult, op1=ALU.mult),
                     reads=ak, writes=[("t1", gq)])
                P.op("dve", lambda e: e.scalar_tensor_tensor(out=t1[:, cs], in0=t1[:, cs], scalar=1.0,
                                                             in1=aall[:, cs], op0=ALU.add, op1=ALU.mult),
                     reads=[("t1", gq)] + ak, writes=[("t1", gq)])
                P.op("dve", lambda e: e.tensor_tensor(out=ga[:, cs], in0=aall[:, cs], in1=gates[:, tt, cs], op=ALU.mult),
                     reads=ak, writes=[("ga", gq)])
                P.op("act", lambda e: e.activation(out=sg[:, cs], in_=t1[:, cs], func=AF.Sigmoid,
                                                   scale=2.0 * 0.7978845608028654),
                     reads=[("t1", gq)], writes=[("sg", gq)])

            def stage2(gq, tt=tt, pb0=pb0):
                cs = slice(gq * GS, (gq + 1) * GS)
                P.op("dve", lambda e: e.tensor_tensor(out=wgt[:, cs], in0=sg[:, cs], in1=ga[:, cs], op=ALU.mult),
                     reads=[("sg", gq), ("ga", gq)], writes=[("wgt", gq)])
                for j, hk in enumerate(range(gq * GS, (gq + 1) * GS)):
                    g = bufs_of[gq][j]
                    db = hk % 8
                    P.op("act", lambda e, db=db, hk=hk: e.activation(out=dg[db], in_=ident[:], func=AF.Copy,
                                                                     scale=wgt[:, hk:hk + 1]),
                         reads=[("wgt", gq), "ident"], writes=[("dg", db)])
                    P.op("pe", mmgroup([(PSv[:, pb0, :], dg[db], UV[g][:, 1024:1536], hk == 0, hk == 127),
                                        (PSv[:, pb0 + 1, :], dg[db], UV[g][:, 1536:2048], hk == 0, hk == 127)]),
                         reads=[("dg", db), ("UV", g)], writes=[("ps", pb0), ("ps", pb0 + 1)])

            for gq in range(ngrp):
                stage1(gq)
                if gq >= 1:
                    stage2(gq - 1)
            stage2(ngrp - 1)
            for half in range(2):
                hs = slice(half * 512, (half + 1) * 512)
                P.op("dve", lambda e, half=half, hs=hs, pb0=pb0: e.tensor_tensor(out=ptmp[half], in0=PSv[:, pb0 + half, :],
                                                                                 in1=G2[:, hs], op=ALU.mult),
                     reads=[("ps", pb0 + half)], writes=[("ptmp", half)])
                P.op("pool", lambda e, half=half, hs=hs, tt=tt: e.tensor_tensor(out=Xv[:, tt, hs], in0=Xv[:, tt, hs],
                                                                                in1=ptmp[half], op=ALU.add),
                     reads=[("ptmp", half), ("x", tt)], writes=[("x", tt)])
        P.fence()
        if dbg and l == dbg_layer:
            P.op("sp", dma(dbg_d["d_x2"], X[:].rearrange("p t d -> p (t d)")), reads=[], writes=["dbg_x2"], dma=True)
            P.fence()

    if late:
        P.op("sp", dma(dbg_d["d_eidx"], YA[:, 0:4096].bitcast(I32)), reads=[], writes=["dbg_e"], dma=True)
        P.op("sp", dma(dbg_d["d_gates"], YA[:, 4096:8192].bitcast(F32)), reads=[], writes=["dbg_g"], dma=True)
    fgr = R1[:, 0:2048].bitcast(F32)
    P.op("sp", dma(fgr, fg_d), writes=["fgr"], dma=True)
    junk = YC[:, 4096:5120]
    for tt in range(NT):
        P.op("act", lambda e, tt=tt: e.activation(out=junk, in_=Xv[:, tt, :], func=AF.Square, accum_out=ss[:, tt:tt + 1]),
             reads=[("x", tt)], writes=["junk", ("ss", tt)])
    P.op("act", lambda e: e.activation(out=srt, in_=ss, func=AF.Sqrt, scale=1.0 / D, bias=1e-6),
         reads=[("ss", tt) for tt in range(NT)], writes=["srt"])
    P.op("dve", lambda e: e.reciprocal(out=rstd, in_=srt), reads=["srt"], writes=["rstd"])
    ycf = YC[:].bitcast(F32)
    obuf = [ycf[:, 0:1024], ycf[:, 1024:2048]]
    okeys = []
    for tt in range(NT):
        ob = tt % 2
        P.op("dve", lambda e, tt=tt, ob=ob: e.scalar_tensor_tensor(out=obuf[ob], in0=Xv[:, tt, :],
                                                                   scalar=rstd[:, tt:tt + 1], in1=fgr, op0=ALU.mult,
                                                                   op1=ALU.mult),
             reads=[("x", tt), "rstd", "fgr"], writes=[("obuf", ob)])
        P.op("sp", dma(out_d[tt * 128:(tt + 1) * 128, :], obuf[ob]), reads=[("obuf", ob)], writes=[("out", tt)], dma=True)
        okeys.append(("out", tt))
    P.op("sp", lambda e: e.nop(), reads=okeys + [k for k in P.state if isinstance(k, str) and k.startswith("dbg")],
         writes=["done"])
    P.emit()
    es.close()
    return nc


def host_consts():
    slopes = np.array([2.0 ** (-8.0 * (h + 1) / 8) for h in range(8)], dtype=np.float64)
    ident = np.eye(128, dtype=np.float32)
    selB = np.zeros((128, 16, 128), np.float32)
    for j in range(16):
        selB[j, j, :] = 1.0
        e = j // 8
        selB[16 + 2 * e, j, :] = 1.0
        selB[17 + 2 * e, j, :] = 1.0
    cm = np.zeros((128, 4, 512), np.float32)
    ik = np.arange(128)[:, None]
    iq = np.arange(512)[None, :]
    for j in range(4):
        cm[:, j, :] = np.where(iq >= 128 * j + ik, 0.0, NEG)
    q = np.arange(T)
    ar = np.zeros((4, 4, T), np.float32)
    for c in range(4):
        for e in range(2):
            h = 2 * c + e
            ar[c, 2 * e, :] = -slopes[h] * 128.0 * (q // 128)
            ar[c, 2 * e + 1, :] = -slopes[h] * (q % 128)
    kb = np.zeros((128, 8, 16), np.float32)
    p = np.arange(128)
    for h in range(8):
        for kt in range(16):
            kb[:, h, kt] = slopes[h] * (128.0 * kt + p)
    iota = np.tile(np.arange(16, dtype=np.float32)[None, :], (128, 1))
    return {"c_ident": ident, "c_selB": selB.reshape(128, 2048), "c_causal": cm.reshape(128, 2048),
            "c_alibi_rows": ar, "c_kb": kb.reshape(128, 128), "c_iota16": iota}


def host_inputs(inputs):
    f = lambda a: np.ascontiguousarray(np.asarray(a, dtype=np.float32))
    shared = {
        "w_ada": f(inputs["w_ada"]),
        "b_ada_rep": f(np.broadcast_to(np.asarray(inputs["b_ada"])[:, None, :], (L, 128, 6 * D))),
        "n1g_rep": f(np.broadcast_to(np.asarray(inputs["norm1_g"])[:, None, :], (L, 128, D))),
        "n2g_rep": f(np.broadcast_to(np.asarray(inputs["norm2_g"])[:, None, :], (L, 128, D))),
        "fg_rep": f(np.broadcast_to(np.asarray(inputs["final_g"])[None, :], (128, D))),
        "w_in": f(inputs["w_in"]),
        "conv_wT": f(np.asarray(inputs["conv_w"]).transpose(0, 2, 1).reshape(L, 4, 128, 3).transpose(0, 2, 1, 3)
                     .reshape(L, 128, 12)),
        "w_attn_proj": f(inputs["w_attn_proj"]),
        "w_conv_proj": f(inputs["w_conv_proj"]),
        "w_out": f(inputs["w_out"]),
        "w_query": f(inputs["w_query"]),
        "keysT": f(np.asarray(inputs["sub_keys"]).transpose(0, 4, 1, 2, 3).reshape(L, 128, 2048)),
    }
    shared["experts_all"] = f(np.concatenate([np.asarray(inputs["expert_u"]).reshape(L * 16384, D),
                                               np.asarray(inputs["expert_v"]).reshape(L * 16384, D)], axis=1))
    shared.update(host_consts())
    x = np.asarray(inputs["x"], dtype=np.float32)
    c = np.asarray(inputs["c"], dtype=np.float32)
    maps = []
    for b in range(x.shape[0]):
        m = dict(shared)
        m["x"] = np.ascontiguousarray(x[b])
        m["crep"] = f(np.broadcast_to(c[b][:, None], (D, 128)))
        maps.append(m)
    return maps


_NC_CACHE = {}


def kernel(**inputs):
    maps = host_inputs(inputs)
    if "nc" not in _NC_CACHE:
        _NC_CACHE["nc"] = build()
    nc = _NC_CACHE["nc"]
    res = run_bass_kernel_spmd(nc, maps, core_ids=list(range(8)))
    out = np.stack([np.asarray(r["out"], dtype=np.float32) for r in res.results], axis=0)
    return out
```
